# Optimizing a Trainium2 kernel written in Bass

```python
import jax, jax.numpy as jnp
from jax import lax
import numpy as np

D_MODEL = 1024
BATCH = 32
SEQ = 2048
DEPTH = 1

GRID_W = 64
CTX_LEN = 256
HEAD_DIM = 64
N_Q_HEADS = (D_MODEL // 2) // HEAD_DIM
N_KV_HEADS = N_Q_HEADS // 4
Q_PER_KV = N_Q_HEADS // N_KV_HEADS
WINDOW = 128
BLOCK = 128
ROPE_FREQS = HEAD_DIM // 4
ROPE_BASE = 10000.0
GLA_HEADS = 4
GLA_DV = (D_MODEL // 2) // GLA_HEADS
GLA_DK = GLA_DV // 2
GATE_RANK = 16
GATE_NORMALIZER = 16.0
CHUNK = 64
N_EXPERTS = 16
CAPACITY_FACTOR = 2
D_EXPERT = D_MODEL
ATTN_WIDTH = N_Q_HEADS * HEAD_DIM
KV_WIDTH = N_KV_HEADS * HEAD_DIM
GLA_QK_WIDTH = GLA_HEADS * GLA_DK
GLA_WIDTH = GLA_HEADS * GLA_DV
MIX_WIDTH = ATTN_WIDTH + GLA_WIDTH
IN_PROJ_DIM = ATTN_WIDTH + 2 * KV_WIDTH + 2 * GLA_QK_WIDTH + 2 * GLA_WIDTH + 2 * GATE_RANK
EPS = 1e-6
NEG_INF = -1e30

kernel_name = "hybrid_swa_gla_ecmoe_diffusion_layer"


def rms_norm(t, g):
    tf = t.astype(jnp.float32)
    y = tf * lax.rsqrt(jnp.mean(tf * tf, axis=-1, keepdims=True) + EPS)
    return (y * g.astype(jnp.float32)).astype(t.dtype)


def modulate(h, shift, scale):
    return h * (1.0 + scale) + shift


def rev(t):
    return jnp.flip(t, axis=1)


def split_in_proj(t):
    sizes = (ATTN_WIDTH, KV_WIDTH, KV_WIDTH, GLA_QK_WIDTH, GLA_QK_WIDTH,
             GLA_WIDTH, GLA_WIDTH, GATE_RANK, GATE_RANK)
    return jnp.split(t, np.cumsum(sizes)[:-1].tolist(), axis=-1)


def heads(t, n, d):
    return t.reshape(t.shape[:2] + (n, d))


def axial_rope_tables(n_rows):
    inv = ROPE_BASE ** (-jnp.arange(ROPE_FREQS, dtype=jnp.float32) / ROPE_FREQS)
    row = jnp.repeat(jnp.arange(n_rows, dtype=jnp.float32), GRID_W)
    col = jnp.tile(jnp.arange(GRID_W, dtype=jnp.float32), n_rows)
    ang = jnp.stack([row[:, None] * inv, col[:, None] * inv], axis=1)
    return jnp.cos(ang), jnp.sin(ang)


def apply_axial_rope(t, cos, sin):
    tt = t.reshape(t.shape[:-1] + (2, 2, ROPE_FREQS))
    t1, t2 = tt[..., 0, :], tt[..., 1, :]
    cs, sn = cos[:, None], sin[:, None]
    out = jnp.stack([t1 * cs - t2 * sn, t2 * cs + t1 * sn], axis=-2)
    return out.reshape(t.shape).astype(t.dtype)


def attn_q(t, g, rope):
    B, T = t.shape[:2]
    q = rms_norm(heads(t, N_Q_HEADS, HEAD_DIM), g)
    if rope is not None:
        q = apply_axial_rope(q, *rope)
    return (q * HEAD_DIM ** -0.5).reshape(B, T, N_KV_HEADS, Q_PER_KV, HEAD_DIM)


def attn_kv(tk, tv, g, rope):
    k = rms_norm(heads(tk, N_KV_HEADS, HEAD_DIM), g)
    if rope is not None:
        k = apply_axial_rope(k, *rope)
    return k, heads(tv, N_KV_HEADS, HEAD_DIM)


def attn_with_sink(q, k, v, sink, mask=None):
    s = jnp.einsum('bqhgd,bkhd->bhgqk', q, k).astype(jnp.float32)
    if mask is not None:
        s = jnp.where(mask, s, NEG_INF)
    sink_logit = jnp.broadcast_to(sink.astype(jnp.float32)[None, :, :, None, None], s.shape[:-1] + (1,))
    p = jax.nn.softmax(jnp.concatenate([sink_logit, s], axis=-1), axis=-1)[..., 1:]
    return jnp.einsum('bhgqk,bkhd->bqhgd', p.astype(v.dtype), v)


def band(t):
    B, L = t.shape[:2]
    tb = t.reshape((B, L // BLOCK, BLOCK) + t.shape[2:])
    tp = jnp.pad(tb, ((0, 0), (1, 1)) + ((0, 0),) * (tb.ndim - 2))
    return jnp.concatenate([tp[:, :-2], tp[:, 1:-1], tp[:, 2:]], axis=2)


def window_attention(q, k, v, kc, vc, sink):
    B, L = q.shape[:2]
    nb = L // BLOCK
    n_ctx = kc.shape[1]
    qb = q.reshape(B, nb, BLOCK, N_KV_HEADS, Q_PER_KV, HEAD_DIM)
    kb, vb = band(k), band(v)
    qi = jnp.arange(BLOCK)
    kj = jnp.arange(3 * BLOCK)
    rel = kj[None, :] - BLOCK - qi[:, None]
    ctx_ok = jnp.ones((BLOCK, n_ctx), dtype=bool)

    def one_block(args):
        n, qn, kn, vn = args
        kpos = (n - 1) * BLOCK + kj
        win = (jnp.abs(rel) <= WINDOW) & ((kpos >= 0) & (kpos < L))[None, :]
        mask = jnp.concatenate([ctx_ok, win], axis=-1)
        return attn_with_sink(qn, jnp.concatenate([kc, kn], axis=1),
                              jnp.concatenate([vc, vn], axis=1), sink, mask)

    o = lax.map(one_block, (jnp.arange(nb), jnp.moveaxis(qb, 1, 0),
                            jnp.moveaxis(kb, 1, 0), jnp.moveaxis(vb, 1, 0)))
    return jnp.moveaxis(o, 0, 1).reshape(B, L, ATTN_WIDTH)


def gla_log_decay(lr, w, b):
    z = (lr @ w + b).astype(jnp.float32)
    return heads(jax.nn.log_sigmoid(z) / GATE_NORMALIZER, GLA_HEADS, GLA_DK)


def gla_chunked(q, k, v, log_a, s0):
    B, T, H, dk = q.shape
    dv = v.shape[-1]
    N = T // CHUNK
    qc = q.astype(jnp.float32).reshape(B, N, CHUNK, H, dk)
    kc = k.astype(jnp.float32).reshape(B, N, CHUNK, H, dk)
    vc = v.astype(jnp.float32).reshape(B, N, CHUNK, H, dv)
    cum = jnp.cumsum(log_a.reshape(B, N, CHUNK, H, dk), axis=2)
    ref = cum[:, :, CHUNK // 2 - 1:CHUNK // 2]
    A = jnp.einsum('bnihk,bnjhk->bnhij', qc * jnp.exp(cum - ref), kc * jnp.exp(ref - cum))
    tril = jnp.tril(jnp.ones((CHUNK, CHUNK), dtype=bool))
    o_intra = jnp.einsum('bnhij,bnjhv->bnihv', jnp.where(tril, A, 0.0), vc)
    total = cum[:, :, -1]
    kv = jnp.einsum('bnjhk,bnjhv->bnhkv', kc * jnp.exp(total[:, :, None] - cum), vc)

    def step(S, inp):
        decay, kv_n = inp
        return decay[..., None] * S + kv_n, S

    s_final, s_enter = lax.scan(step, s0, (jnp.moveaxis(jnp.exp(total), 1, 0), jnp.moveaxis(kv, 1, 0)))
    s_enter = jnp.moveaxis(s_enter, 0, 1)
    o_inter = jnp.einsum('bnihk,bnhkv->bnihv', qc * jnp.exp(cum), s_enter)
    return (o_intra + o_inter).reshape(B, T, H, dv).astype(v.dtype), s_final


def gla_final_state(k, v, log_a):
    cum = jnp.cumsum(log_a, axis=1)
    w = k.astype(jnp.float32) * jnp.exp(cum[:, -1:] - cum)
    return jnp.einsum('bthk,bthv->bhkv', w, v.astype(jnp.float32))


def gla_bidirectional(q, k, v, la_f, la_b, s_f0, s_b0):
    o_f, s_f = gla_chunked(q, k, v, la_f, s_f0)
    o_b, s_b = gla_chunked(rev(q), rev(k), rev(v), rev(la_b), s_b0)
    return o_f + rev(o_b), s_f, s_b


def gla_output(o, gate, g):
    B, T = o.shape[:2]
    return rms_norm(o, g).reshape(B, T, GLA_WIDTH) * jax.nn.silu(gate)


def expert_choice_ffn(h, w_router, w_gate, w_up, w_down):
    B, n, _ = h.shape
    cap = CAPACITY_FACTOR * n // N_EXPERTS
    aff = jax.nn.softmax((h @ w_router).astype(jnp.float32), axis=-1)
    g, idx = lax.top_k(jnp.swapaxes(aff, 1, 2), cap)
    bidx = jnp.arange(B)[:, None, None]
    xs = h[bidx, idx]
    hid = jax.nn.silu(jnp.einsum('becd,edf->becf', xs, w_gate)) * jnp.einsum('becd,edf->becf', xs, w_up)
    y = jnp.einsum('becf,efd->becd', hid, w_down) * g[..., None].astype(h.dtype)
    return jnp.zeros_like(h).at[bidx, idx].add(y)


def setup_inputs(seed: int = 0) -> dict:
    key = jax.random.key(seed)
    ks = jax.random.split(key, 22)
    D = D_MODEL

    def nrm(k, shape, scale):
        return scale * jax.random.normal(k, shape, jnp.float32)

    return {
        "x": nrm(ks[0], (BATCH, SEQ, D), 1.0),
        "c": nrm(ks[1], (BATCH, D), 1.0),
        "ctx": nrm(ks[2], (BATCH, CTX_LEN, D), 1.0),
        "c_ctx": nrm(ks[3], (D,), 1.0),
        "w_mod": nrm(ks[4], (DEPTH, D, 6 * D), 0.5 * D ** -0.5),
        "b_mod": nrm(ks[5], (DEPTH, 6 * D), 0.01),
        "norm1_g": 1.0 + nrm(ks[6], (DEPTH, D), 0.02),
        "w_in": nrm(ks[7], (DEPTH, D, IN_PROJ_DIM), D ** -0.5),
        "q_norm_g": 1.0 + nrm(ks[8], (DEPTH, HEAD_DIM), 0.02),
        "k_norm_g": 1.0 + nrm(ks[9], (DEPTH, HEAD_DIM), 0.02),
        "attn_sink": nrm(ks[10], (DEPTH, N_Q_HEADS), 0.5),
        "w_decay_fwd": nrm(ks[11], (DEPTH, GATE_RANK, GLA_QK_WIDTH), GATE_RANK ** -0.5),
        "b_decay_fwd": nrm(ks[12], (DEPTH, GLA_QK_WIDTH), 0.1),
        "w_decay_bwd": nrm(ks[13], (DEPTH, GATE_RANK, GLA_QK_WIDTH), GATE_RANK ** -0.5),
        "b_decay_bwd": nrm(ks[14], (DEPTH, GLA_QK_WIDTH), 0.1),
        "gla_norm_g": 1.0 + nrm(ks[15], (DEPTH, GLA_DV), 0.02),
        "w_out": nrm(ks[16], (DEPTH, MIX_WIDTH, D), MIX_WIDTH ** -0.5),
        "norm2_g": 1.0 + nrm(ks[17], (DEPTH, D), 0.02),
        "w_router": nrm(ks[18], (DEPTH, D, N_EXPERTS), D ** -0.5),
        "w_e_gate": nrm(ks[19], (DEPTH, N_EXPERTS, D, D_EXPERT), D ** -0.5),
        "w_e_up": nrm(ks[20], (DEPTH, N_EXPERTS, D, D_EXPERT), D ** -0.5),
        "w_e_down": nrm(ks[21], (DEPTH, N_EXPERTS, D_EXPERT, D), D_EXPERT ** -0.5),
    }


def reference(x, c, ctx, c_ctx, w_mod, b_mod, norm1_g, w_in, q_norm_g, k_norm_g, attn_sink,
              w_decay_fwd, b_decay_fwd, w_decay_bwd, b_decay_bwd, gla_norm_g, w_out, norm2_g,
              w_router, w_e_gate, w_e_up, w_e_down):
    B, L, _ = x.shape
    n_ctx = ctx.shape[1]
    n_rows = L // GRID_W
    rope = axial_rope_tables(n_rows)
    for layer in range(DEPTH):
        last = layer == DEPTH - 1
        mod = jax.nn.silu(c) @ w_mod[layer] + b_mod[layer]
        sh1, sc1, g1, sh2, sc2, g2 = jnp.split(mod[:, None, :], 6, axis=-1)
        mod_c = jax.nn.silu(c_ctx) @ w_mod[layer] + b_mod[layer]
        sh1c, sc1c, g1c, sh2c, sc2c, g2c = jnp.split(mod_c, 6, axis=-1)
        w_aq, w_ak, w_av, w_gq, w_gk, w_gv, w_gg, w_lf, w_lb = split_in_proj(w_in[layer])
        sink = attn_sink[layer].reshape(N_KV_HEADS, Q_PER_KV)

        hc = modulate(rms_norm(ctx, norm1_g[layer]), sh1c, sc1c)
        kc, vc = attn_kv(hc @ w_ak, hc @ w_av, k_norm_g[layer], None)
        gkc = heads(hc @ w_gk, GLA_HEADS, GLA_DK)
        gvc = heads(hc @ w_gv, GLA_HEADS, GLA_DV)
        la_fc = gla_log_decay(hc @ w_lf, w_decay_fwd[layer], b_decay_fwd[layer])
        la_bc = gla_log_decay(hc @ w_lb, w_decay_bwd[layer], b_decay_bwd[layer])
        if last:
            s_f = gla_final_state(gkc, gvc, la_fc)
            s_b = gla_final_state(rev(gkc), rev(gvc), rev(la_bc))
        else:
            zero_state = jnp.zeros((B, GLA_HEADS, GLA_DK, GLA_DV), jnp.float32)
            gqc = heads(hc @ w_gq, GLA_HEADS, GLA_DK) * GLA_DK ** -0.5
            gla_c, s_f, s_b = gla_bidirectional(gqc, gkc, gvc, la_fc, la_bc, zero_state, zero_state)
            qc = attn_q(hc @ w_aq, q_norm_g[layer], None)
            attn_c = attn_with_sink(qc, kc, vc, sink).reshape(B, n_ctx, ATTN_WIDTH)
            mix_c = jnp.concatenate([attn_c, gla_output(gla_c, hc @ w_gg, gla_norm_g[layer])], axis=-1)

        h = modulate(rms_norm(x, norm1_g[layer]), sh1, sc1)
        aq, ak, av, gq, gk, gv, gg, lf, lb = split_in_proj(h @ w_in[layer])
        q = attn_q(aq, q_norm_g[layer], rope)
        k, v = attn_kv(ak, av, k_norm_g[layer], rope)
        attn_lat = window_attention(q, k, v, kc, vc, sink)
        la_f = gla_log_decay(lf, w_decay_fwd[layer], b_decay_fwd[layer])
        la_b = gla_log_decay(lb, w_decay_bwd[layer], b_decay_bwd[layer])
        gla_lat, _, _ = gla_bidirectional(heads(gq, GLA_HEADS, GLA_DK) * GLA_DK ** -0.5,
                                          heads(gk, GLA_HEADS, GLA_DK), heads(gv, GLA_HEADS, GLA_DV),
                                          la_f, la_b, s_f, s_b)
        mix = jnp.concatenate([attn_lat, gla_output(gla_lat, gg, gla_norm_g[layer])], axis=-1)
        x = x + g1 * (mix @ w_out[layer])
        h2 = modulate(rms_norm(x, norm2_g[layer]), sh2, sc2)
        x = x + g2 * expert_choice_ffn(h2, w_router[layer], w_e_gate[layer], w_e_up[layer], w_e_down[layer])

        if not last:
            ctx = ctx + g1c * (mix_c @ w_out[layer])
            h2c = modulate(rms_norm(ctx, norm2_g[layer]), sh2c, sc2c)
            ctx = ctx + g2c * expert_choice_ffn(h2c, w_router[layer], w_e_gate[layer], w_e_up[layer], w_e_down[layer])
    return x
```

```python
import contextlib
import os
import numpy as np
BIS = int(os.environ.get('BIS', '99'))
OPT_REC = int(os.environ.get('OPT_REC', '0'))
OPT_CPREP = int(os.environ.get('OPT_CPREP', '1'))
OPT_REORD = int(os.environ.get('OPT_REORD', '1'))
OPT_RT = int(os.environ.get('OPT_RT', '0'))
OPT_I16 = int(os.environ.get('OPT_I16', '1'))
OA1 = int(os.environ.get('OA1', '0'))
OA4 = int(os.environ.get('OA4', '1'))
OB1 = int(os.environ.get('OB1', '0'))
OC7 = int(os.environ.get('OC7', '1'))
OC8 = int(os.environ.get('OC8', '1'))
OAP = int(os.environ.get('OAP', '0'))
OG2 = int(os.environ.get('OG2', '0'))
OP0 = int(os.environ.get('OP0', '0'))
OM1 = int(os.environ.get('OM1', '1'))
OCL = int(os.environ.get('OCL', '1'))
OC9 = int(os.environ.get('OC9', '0'))
OATT = int(os.environ.get('OATT', '3'))
OH7 = int(os.environ.get('OH7', '0'))
NOSELF = os.environ.get('NOSELF', '')
OB2 = int(os.environ.get('OB2', '0'))
OB3 = int(os.environ.get('OB3', '0'))
OB4 = int(os.environ.get('OB4', '0'))
OB5 = int(os.environ.get('OB5', '0'))
OA5 = int(os.environ.get('OA5', '0'))
OA6 = int(os.environ.get('OA6', '0'))
import concourse.bass as bass
import concourse.mybir as mybir
from concourse.alu_op_type import AluOpType as ALU
from concourse.bass_utils import run_bass_kernel_spmd

AF = mybir.ActivationFunctionType
AX = mybir.AxisListType
F32 = mybir.dt.float32
BF16 = mybir.dt.bfloat16
I32 = mybir.dt.int32
I16 = mybir.dt.int16

NCORES = 8
D = 1024
T = 2048
C = 256
S = T + C
NE = 16
CAP = 256
EPS = 1e-6

CENG = ('pe', 'act', 'dve', 'pool')
ALLENG = ('pe', 'act', 'dve', 'pool', 'sp')


class Prog:
    def __init__(self, nc):
        self.nc = nc
        self.ops = {e: [] for e in ALLENG}
        self.cnt = {e: 0 for e in CENG}
        self.seen = {e: {} for e in ALLENG}
        self.last_w = {}
        self.readers = {}
        lanes_per_queue = {'sp': 14, 'pool': 14, 'act': 2, 'poolw': 12}
        self.lanes = {q: ['L_%s_%d' % (q, i) for i in range(n)] for q, n in lanes_per_queue.items()}
        self.lane_cnt = {l: 0 for q in self.lanes for l in self.lanes[q]}
        self.lane_rr = {q: 0 for q in self.lanes}
        self.semnames = ['E_' + e for e in CENG] + [l for q in self.lanes for l in self.lanes[q]]
        self.sems = None
        self.flushed = {e: 0 for e in ALLENG}
        self.scoped_bases = set()
        self.cls = {}
        self.live_scoped = set()
        self.fence = {}

    def _touch(self, names):
        for n in names:
            c = self.cls.get(n)
            if c is None:
                c = any(n.startswith(b_) for b_ in self.scoped_bases)
                self.cls[n] = c
            if c and n not in self.live_scoped:
                self.live_scoped.add(n)
                self.last_w.pop(n, None)
                self.readers[n] = dict(self.fence)

    def end_scope(self):
        for e in CENG:
            if self.cnt[e] > 0:
                self.fence['E_' + e] = self.cnt[e]
        for l, c in self.lane_cnt.items():
            if c > 0:
                self.fence[l] = 16 * c
        for n in self.live_scoped:
            self.last_w.pop(n, None)
            self.readers.pop(n, None)
        self.live_scoped = set()

    def _deps(self, eng, reads, writes):
        self._touch(reads)
        self._touch(writes)
        need = {}

        def add(tok):
            if tok is None:
                return
            s, v = tok
            if eng == 'pe' and s == 'E_pe':
                return
            if NOSELF and eng in NOSELF.split(',') and s == 'E_' + eng:
                return
            if need.get(s, 0) < v:
                need[s] = v
        for r in reads:
            add(self.last_w.get(r))
        for w in writes:
            add(self.last_w.get(w))
            for t in self.readers.get(w, {}).items():
                add(t)
        return need

    def _commit(self, tok, reads, writes):
        for w in writes:
            self.last_w[w] = tok
            self.readers[w] = {}
        for r in reads:
            if r in writes:
                continue
            d = self.readers.setdefault(r, {})
            if d.get(tok[0], 0) < tok[1]:
                d[tok[0]] = tok[1]

    def _filter(self, eng, need):
        out = []
        seen = self.seen[eng]
        for s, v in need.items():
            if seen.get(s, 0) < v:
                seen[s] = v
                out.append((s, v))
        return out

    def op(self, eng, fn, reads=(), writes=()):
        need = self._deps(eng, reads, writes)
        waits = self._filter(eng, need)
        self.cnt[eng] += 1
        tok = ('E_' + eng, self.cnt[eng])
        self.ops[eng].append((waits, fn, tok[0], 1))
        self._commit(tok, reads, writes)
        return tok

    def I(self, eng, method, *args, reads=(), writes=(), **kw):
        return self.op(eng, lambda e: getattr(e, method)(*args, **kw), reads, writes)

    def dma(self, q0, fn, reads=(), writes=()):
        q = q0
        q0 = 'pool' if q == 'poolw' else q
        need = self._deps(q0, reads, writes)
        lane = self.lanes[q][self.lane_rr[q] % len(self.lanes[q])]
        self.lane_rr[q] += 1
        prior = 16 * self.lane_cnt[lane]
        if prior > 0 and need.get(lane, 0) < prior:
            need[lane] = prior
        waits = self._filter(q0, need)
        self.lane_cnt[lane] += 1
        tok = (lane, prior + 16)
        self.ops[q0].append((waits, fn, lane, 16))
        self._commit(tok, reads, writes)
        return tok

    def DMA(self, q, out, in_, reads=(), writes=(), **kw):
        return self.dma(q, lambda e: e.dma_start(out=out, in_=in_, **kw), reads, writes)

    def wait_all(self, eng):
        need = {}
        for e in CENG:
            if e != eng and self.cnt[e] > 0:
                need['E_' + e] = self.cnt[e]
        for l, c in self.lane_cnt.items():
            if c > 0:
                need[l] = 16 * c
        waits = self._filter(eng, need)
        if waits:
            self.ops[eng].append((waits, None, None, 0))

    def barrier(self):
        for e in ALLENG:
            self.wait_all(e)

    def alloc_sems(self, es):
        self.sems = {n: es.enter_context(self.nc.semaphore(n)) for n in self.semnames}

    def emit(self):
        nc = self.nc
        prog = self

        def replay(ename, eng):
            lst = prog.ops[ename]
            for waits, fn, sname, inc in lst[prog.flushed[ename]:]:
                for s, v in waits:
                    eng.wait_ge(prog.sems[s], v)
                if fn is not None:
                    ins = fn(eng)
                    ins.then_inc(prog.sems[sname], inc)
            prog.flushed[ename] = len(lst)

        with nc.Block() as block:
            @block.tensor
            def _(eng):
                replay('pe', eng)

            @block.scalar
            def _(eng):
                replay('act', eng)

            @block.vector
            def _(eng):
                replay('dve', eng)

            @block.gpsimd
            def _(eng):
                replay('pool', eng)

            @block.sync
            def _(eng):
                replay('sp', eng)


def host_consts():
    p = np.arange(128)
    d = p % 64
    axis = d // 32
    half = (d % 32) // 16
    freq = d % 16
    inv = (np.float32(10000.0) ** (-(freq.astype(np.float32)) / np.float32(16))).astype(np.float32)
    t = np.arange(T)
    row = (t // 64).astype(np.float32)
    col = (t % 64).astype(np.float32)
    pos = np.where(axis[:, None] == 0, row[None, :], col[None, :]).astype(np.float32)
    ang = (pos * inv[:, None]).astype(np.float32)
    cos = np.ones((128, S), np.float32)
    sin = np.zeros((128, S), np.float32)
    cos[:, C:] = np.cos(ang)
    sgn = np.where(half == 0, -1.0, 1.0).astype(np.float32)
    sin[:, C:] = np.sin(ang) * sgn[:, None]
    partner = (p // 64) * 64 + np.where(half == 0, d + 16, d - 16)
    perm = np.zeros((128, 128), np.float32)
    perm[partner, p] = 1.0
    bones = (p[:, None] // 64 == p[None, :] // 64).astype(np.float32)
    ident = np.eye(128, dtype=np.float32)
    i = p[:, None]
    q = p[None, :]
    mattn = np.stack([(i >= q), (i <= q)], axis=1).astype(np.float32)
    same = (i // 64 == q // 64)
    mgla = np.stack([same & (i <= q), same & (i >= q)], axis=1).astype(np.float32)
    rmask = np.ones((128, 512), np.float32)
    rmask[:, ::64] = 0.0
    slotid = np.stack([p, p + 128], axis=1).astype(np.float32)
    return dict(c_cos=cos, c_sin=sin, c_perm=perm, c_bones=bones, c_ident=ident,
                c_mattn=np.ascontiguousarray(mattn), c_mgla=np.ascontiguousarray(mgla),
                c_rmask=rmask, c_slotid=slotid)


def build(NB, stage=99, dumps=()):
    nc = bass.Bass("TRN2", target_bir_lowering=False)
    R = NB * NE
    NJ = NB * 2
    NSL = NB * CAP

    def din(name, shape, dt=F32):
        return nc.dram_tensor(name, list(shape), dt, kind="ExternalInput").ap()

    x = din("x", [NB, T, D])
    ctx = din("ctx", [NB, C, D])
    cT = din("cT", [128, 8, NB + 1])
    w_mod = din("w_mod", [D, 6 * D])
    b_modT = din("b_modT", [128, 48])
    n1gT = din("n1gT", [128, 8])
    w_in = din("w_in", [D, 2336])
    qng = din("qng", [64, 1])
    kng = din("kng", [64, 1])
    sink = din("sink", [8])
    wdf = din("wdf", [16, 256])
    wdb = din("wdb", [16, 256])
    bdec = din("bdec", [128, 2, 2])
    glag = din("glag", [128])
    w_out = din("w_out", [D, D])
    n2g = din("n2g", [D])
    w_router = din("w_router", [D, NE])
    weg = din("weg", [NE, D, D])
    weu = din("weu", [NE, D, D])
    wed = din("wed", [NE, D, D])
    c_cos = din("c_cos", [128, S])
    c_sin = din("c_sin", [128, S])
    c_perm = din("c_perm", [128, 128])
    c_bones = din("c_bones", [128, 128])
    c_ident = din("c_ident", [128, 128])
    c_mattn = din("c_mattn", [128, 2, 128])
    c_mgla = din("c_mgla", [128, 2, 128])
    c_rmask = din("c_rmask", [128, 512])
    c_slotid = din("c_slotid", [128, 2])
    out = nc.dram_tensor("out", [NB, T, D], F32, kind="ExternalOutput").ap()
    outf = out.rearrange("b t d -> (b t) d")
    h2d = nc.dram_tensor("h2d", [NB * T, D], BF16, kind="Internal").ap()
    affd = nc.dram_tensor("affd", [NB * T, NE], F32, kind="Internal").ap()
    cumd = nc.dram_tensor("cumd", [R, T], I16 if OPT_I16 else F32, kind="Internal").ap()

    P = Prog(nc)
    dump_aps = {}

    def dump(name, src_ap, shape, dt, reads):
        if name not in dumps:
            return
        dd = nc.dram_tensor("dbg_" + name, list(shape), dt, kind="ExternalOutput").ap()
        P.DMA('sp', dd, src_ap, reads=reads, writes=['dbg_' + name])

    with contextlib.ExitStack() as es:
        P.alloc_sems(es)

        uniq = [0]
        persist_es = [None]

        def sb(es_, name, shape, dt):
            uniq[0] += 1
            if es_ is not es and es_ is not persist_es[0]:
                P.scoped_bases.add(name)
            return es_.enter_context(nc.sbuf_tensor("%s_%d" % (name, uniq[0]), list(shape), dt))

        PP = [es.enter_context(nc.psum_tensor("PP%d" % i, [128, 1024], F32)) for i in range(2)]
        BK = [es.enter_context(nc.psum_tensor("BK%d" % i, [128, 512], F32)) for i in range(4)]
        banks = []
        for i in range(2):
            banks.append((PP[i][:, 0:512], 'PP%da' % i))
            banks.append((PP[i][:, 512:1024], 'PP%db' % i))
        for i in range(4):
            banks.append((BK[i][:, :], 'BK%d' % i))
        PPb = [PP[i].bitcast(BF16) for i in range(2)]
        BKb = [BK[i].bitcast(BF16) for i in range(4)]

        ident_b = sb(es, "ident_b", [128, 128], BF16)
        ident_f = sb(es, "ident_f", [128, 128], F32)
        modT = sb(es, "modT", [128, 48, NB + 1], F32)
        aff = sb(es, "aff", [128, NB, 16, NE], F32)
        affT = sb(es, "affT", [R, T], F32)
        P.DMA('pool', ident_b[:], c_ident, writes=['ident_b'])
        P.DMA('sp', ident_f[:], c_ident, writes=['ident_f'])

        with contextlib.ExitStack() as e0:
            cs_f = sb(e0, "cs_f", [128, 8, NB + 1], F32)
            cs = sb(e0, "cs", [128, 8, NB + 1], BF16)
            bmT = sb(e0, "bmT", [128, 48], F32)
            wm = [sb(e0, "wm%d" % i, [128, 8, 1024], BF16) for i in range(2)]
            P.DMA('sp', cs_f[:], cT, writes=['cs_f'])
            P.DMA('sp', bmT[:], b_modT, writes=['bmT'])
            P.I('act', 'activation', out=cs[:], in_=cs_f[:], func=AF.Silu, reads=['cs_f'], writes=['cs'])
            if OP0:
                csf = sb(e0, "csf", [128, 8, NB + 1], F32)
                wmf = sb(e0, "wmf", [128, 8, 1024], F32)
                P.I('act', 'activation', out=csf[:], in_=cs_f[:], func=AF.Silu, reads=['cs_f'], writes=['csf'])
            for j in range(6):
                hw = OP0 and (j % 2 == 1)
                if hw:
                    w_, wn, rhs_, rn = wmf, 'wmf', csf, 'csf'
                else:
                    ib = (j // 2) % 2 if OP0 else j % 2
                    w_, wn, rhs_, rn = wm[ib], 'wm%d' % ib, cs, 'cs'
                for k2 in range(2):
                    P.DMA('sp' if hw else 'pool', w_[:, 4 * k2:4 * k2 + 4, :],
                          w_mod[k2 * 512:(k2 + 1) * 512, j * 1024:(j + 1) * 1024].rearrange("(k p) c -> p k c", p=128),
                          writes=[wn + '_k%d' % k2])
                pm, pmn = banks[4 + (j % 2)]
                pmv = pm[:, 0:64].rearrange("p (f c) -> p f c", c=8)
                for fc in range(8):
                    for kc in range(8):
                        P.I('pe', 'matmul', pmv[:, fc, 0:NB + 1], w_[:, kc, fc * 128:(fc + 1) * 128], rhs_[:, kc, :],
                            start=(kc == 0), stop=(kc == 7), reads=[wn + '_k0', wn + '_k1', rn], writes=[pmn])
                P.I('dve', 'tensor_tensor', out=modT[:, j * 8:(j + 1) * 8, :], in0=pmv[:, :, 0:NB + 1],
                    in1=bmT[:, j * 8:(j + 1) * 8].unsqueeze(2).to_broadcast([128, 8, NB + 1]), op=ALU.add,
                    reads=[pmn, 'bmT'], writes=['modT'])
            P.barrier()
        dump("modT", modT[:], [128, 48, NB + 1], F32, ['modT'])

        with contextlib.ExitStack() as em:
            persist_es[0] = em
            perm_b = sb(em, "perm_b", [128, 128], BF16)
            bones_b = sb(em, "bones_b", [128, 128], BF16)
            mattn = sb(em, "mattn", [128, 2, 128], BF16)
            mgla = sb(em, "mgla", [128, 2, 128], BF16)
            rmask = sb(em, "rmask", [128, 512], BF16)
            n1g = sb(em, "n1g", [128, 8], F32)
            scale1 = sb(em, "scale1", [128, 8, NB + 1], F32)
            gqc = sb(em, "gqc", [128, 1], F32)
            gkc = sb(em, "gkc", [128, 1], F32)
            es_t = sb(em, "es_t", [128, 8, 1], F32)
            nbd = sb(em, "nbd", [128, 2, 2], F32)
            glab = sb(em, "glab", [128, 128], F32)
            wr = sb(em, "wr", [128, 8, NE], F32)
            wdx = sb(em, "wdx", [32, 2, 256], BF16)
            hT = sb(em, "hT", [128, 8, S], BF16)
            mixT = sb(em, "mixT", [128, 8, T], BF16)
            wsl = sb(em, "wsl", [128, 8, 1024], BF16)
            rep = [sb(em, "rep%d" % i, [128, 128], F32) for i in range(2)]

            for dst, src, nm in ((perm_b, c_perm, 'perm_b'), (bones_b, c_bones, 'bones_b'),
                                 (mattn, c_mattn, 'mattn'), (mgla, c_mgla, 'mgla'), (rmask, c_rmask, 'rmask')):
                P.DMA('pool', dst[:], src, writes=[nm])
            P.DMA('sp', n1g[:], n1gT, writes=['n1g'])
            P.DMA('sp', gqc[0:64, :], qng, writes=['gqc'])
            P.DMA('sp', gqc[64:128, :], qng, writes=['gqc'])
            P.DMA('sp', gkc[0:64, :], kng, writes=['gkc'])
            P.DMA('sp', gkc[64:128, :], kng, writes=['gkc'])
            P.DMA('sp', es_t[:].rearrange("p h o -> p (h o)"), sink.partition_broadcast(128), writes=['es_t'])
            P.DMA('sp', nbd[:], bdec, writes=['nbd'])
            P.DMA('sp', glab[:], glag.partition_broadcast(128), writes=['glab'])
            P.DMA('sp', wr[:], w_router.rearrange("(k p) e -> p k e", p=128), writes=['wr'])
            P.I('dve', 'memset', wdx[:], 0.0, writes=['wdx'])
            P.DMA('pool', wdx[0:16, 0, :], wdf, writes=['wdx'])
            P.DMA('pool', wdx[16:32, 1, :], wdb, writes=['wdx'])
            P.I('dve', 'tensor_scalar', out=gqc[:], in0=gqc[:], scalar1=0.125, scalar2=None, op0=ALU.mult, reads=['gqc'], writes=['gqc'])
            P.I('act', 'activation', out=es_t[:], in_=es_t[:], func=AF.Exp, reads=['es_t'], writes=['es_t'])
            P.I('dve', 'tensor_scalar', out=nbd[:], in0=nbd[:], scalar1=-1.0, scalar2=None, op0=ALU.mult, reads=['nbd'], writes=['nbd'])
            P.I('dve', 'tensor_scalar', out=scale1[:], in0=modT[:, 8:16, :], scalar1=1.0, scalar2=None, op0=ALU.add,
                reads=['modT'], writes=['scale1'])
            P.I('dve', 'tensor_tensor', out=scale1[:], in0=scale1[:], in1=n1g[:].unsqueeze(2).to_broadcast([128, 8, NB + 1]),
                op=ALU.mult, reads=['scale1', 'n1g'], writes=['scale1'])

            repi = [0]

            def bcast_row(dst, dstname, chunk0, col):
                for half in range(2):
                    bk, bkn = banks[6 + half]
                    for q4 in range(4):
                        kc = half * 4 + q4
                        r_ = rep[repi[0] % 2]
                        rn = 'rep%d' % (repi[0] % 2)
                        repi[0] += 1
                        P.I('dve', 'tensor_copy', out=r_[:], in_=modT[:, chunk0 + kc, col:col + 1].to_broadcast([128, 128]),
                            reads=['modT'], writes=[rn])
                        P.I('pe', 'matmul', bk[:, q4 * 128:(q4 + 1) * 128], r_[:], ident_f[:], start=True, stop=True,
                            reads=[rn, 'ident_f'], writes=[bkn])
                    P.I('act', 'activation', out=dst[:, half * 512:(half + 1) * 512], in_=bk, func=AF.Copy,
                        reads=[bkn], writes=[dstname])

            g1bc = sb(em, "g1bc", [128, D], F32)
            wst = [sb(em, "wst%d" % i, [128, D], F32) for i in range(2)]

            def c_prep(b):
                wO = wsl
                bcast_row(g1bc, 'g1bc', 16, b)
                for kc in range(8):
                    P.DMA('sp', wst[kc % 2][:], w_out[kc * 128:(kc + 1) * 128, :], writes=['wst%d' % (kc % 2)])
                    P.I('dve' if kc % 2 == 0 else 'pool', 'tensor_tensor', out=wO[:, kc, :], in0=wst[kc % 2][:], in1=g1bc[:], op=ALU.mult,
                        reads=['wst%d' % (kc % 2), 'g1bc'], writes=['wsl'])

            def h_partA(b, ti):
                xt_ = xt[ti % 2]
                xtn = 'xt%d' % (ti % 2)
                xn_ = xn[ti % 2]
                xnn = 'xn%d' % (ti % 2)
                st_ = st4[ti % 2]
                stn = 'st4_%d' % (ti % 2)
                src = ctx[b, ti * 128:(ti + 1) * 128, :] if ti < 2 else x[b, (ti - 2) * 128:(ti - 1) * 128, :]
                P.DMA('sp', xt_[:], src, writes=[xtn])
                P.I('act', 'activation', out=xn_[:], in_=xt_[:], func=AF.Square, accum_out=st_[:, 0:1],
                    reads=[xtn], writes=[xnn, stn])
                P.I('act', 'activation', out=st_[:, 1:2], in_=st_[:, 0:1], func=AF.Ln, scale=1.0 / D, bias=EPS,
                    reads=[stn], writes=[stn])
                P.I('act', 'activation', out=st_[:, 2:3], in_=st_[:, 1:2], func=AF.Exp, scale=-0.5,
                    reads=[stn], writes=[stn])
                if OH7:
                    return
                h_partA2(b, ti)

            def h_partA2(b, ti):
                xt_ = xt[ti % 2]
                xtn = 'xt%d' % (ti % 2)
                xn_ = xn[ti % 2]
                xnn = 'xn%d' % (ti % 2)
                st_ = st4[ti % 2]
                stn = 'st4_%d' % (ti % 2)
                P.I('dve', 'tensor_scalar', out=xn_[:], in0=xt_[:], scalar1=st_[:, 2:3], scalar2=None, op0=ALU.mult,
                    reads=[xtn, stn], writes=[xnn])
                tpn = 'BK%d' % (2 + ti % 2)
                tp = BKb[2 + ti % 2][:, 0:1024].rearrange("p (k t) -> p k t", t=128)
                for kc in range(8):
                    P.I('pe', 'transpose', tp[:, kc, :], xn_[:, kc * 128:(kc + 1) * 128], ident_b[:],
                        reads=[xnn, 'ident_b'], writes=[tpn])

            def h_partB(b, ti):
                col = NB if ti < 2 else b
                tpn = 'BK%d' % (2 + ti % 2)
                tp = BKb[2 + ti % 2][:, 0:1024].rearrange("p (k t) -> p k t", t=128)
                for kc in range(8):
                    if kc < 3:
                        P.I('act', 'activation', out=hT[:, kc, ti * 128:(ti + 1) * 128], in_=tp[:, kc, :], func=AF.Identity,
                            scale=scale1[:, kc, col:col + 1], bias=modT[:, kc, col:col + 1],
                            reads=[tpn, 'scale1', 'modT'], writes=['hT%d' % ti])
                    else:
                        P.I('dve', 'tensor_scalar', out=hT[:, kc, ti * 128:(ti + 1) * 128], in0=tp[:, kc, :],
                            scalar1=scale1[:, kc, col:col + 1], scalar2=modT[:, kc, col:col + 1], op0=ALU.mult, op1=ALU.add,
                            reads=[tpn, 'scale1', 'modT'], writes=['hT%d' % ti])

            for b in range(NB):
                with contextlib.ExitStack() as eh:
                    xt = [sb(eh, "xt%d" % i, [128, D], F32) for i in range(2)]
                    xn = [sb(eh, "xn%d" % i, [128, D], BF16) for i in range(2)]
                    st4 = [sb(eh, "st4_%d" % i, [128, 4], F32) for i in range(2)]
                    for ti in range(19):
                        if ti < 18:
                            h_partA(b, ti)
                        if ti >= 1:
                            h_partB(b, ti - 1)
                        if ti < 18 and OH7:
                            h_partA2(b, ti)
                    P.end_scope()
                if b == 0:
                    dump("hT", hT[:], [128, 8, S], BF16, ['hT%d' % i for i in range(18)])
                if stage <= 1:
                    continue
                hTall = ['hT%d' % i for i in range(18)]
                chunks = [(0, 256)] + [(C + 512 * i, 512) for i in range(4)]

                with contextlib.ExitStack() as ea:
                    qT = sb(ea, "qT", [128, 4, T], BF16)
                    kT = sb(ea, "kT", [128, 2, S], BF16)
                    vaug = sb(ea, "vaug", [128, 18, 2, 66], BF16)
                    u_ = [sb(ea, "u%d" % i, [128, 512], BF16) for i in range(3 if OAP else 2)]
                    sq_ = [sb(ea, "sq%d" % i, [128, 512], BF16) for i in range(3 if OAP else 2)]
                    lnt = sb(ea, "lnt", [128, 512], F32)
                    rr_ = [sb(ea, "rr%d" % i, [128, 512], F32) for i in range(3 if OAP else 2)]
                    t1_ = [sb(ea, "t1_%d" % i, [128, 512], F32) for i in range(3 if OAP else 2)]
                    t2_ = [sb(ea, "t2_%d" % i, [128, 512], F32) for i in range(3 if OAP else 2)]
                    pt_ = [sb(ea, "pt%d" % i, [128, 640], BF16) for i in range((max(OATT, 2) + 1) if OATT else 2)]
                    den = sb(ea, "den", [128, 8, 1], F32)
                    rec = sb(ea, "rec", [128, 8, 1], F32)
                    ao = sb(ea, "ao", [128, 8, 64], BF16)
                    cos_b = sb(ea, "cos_b", [128, S], BF16)
                    sin_b = sb(ea, "sin_b", [128, S], BF16)
                    for dst, src, nm in ((cos_b, c_cos, 'cos_b'), (sin_b, c_sin, 'sin_b')):
                        P.DMA('pool', dst[:], src, writes=[nm])
                    wA = wsl
                    P.DMA('pool', wA[:, :, 0:512], w_in[:, 0:512].rearrange("(k p) c -> p k c", p=128), writes=['wsl'])
                    for hh in range(2):
                        for dup in range(2):
                            o = 512 + hh * 128 + dup * 64
                            P.DMA('pool', wA[:, :, o:o + 64], w_in[:, 512 + hh * 64:512 + (hh + 1) * 64].rearrange("(k p) c -> p k c", p=128),
                                  writes=['wsl'])
                    P.DMA('pool', wA[:, :, 768:896], w_in[:, 640:768].rearrange("(k p) c -> p k c", p=128), writes=['wsl'])
                    P.I('pool', 'memset', vaug[:, :, :, 64:65], 1.0, writes=['vaug'])
                    def a_p1(s0, n, mc, i2, ib=None):
                        tiles = list(range(s0 // 128, (s0 + n) // 128))
                        hTr = ['hT%d' % i for i in tiles]
                        pj, pjn = banks[i2]
                        i2 = i2 if ib is None else ib
                        for kc in range(8):
                            P.I('pe', 'matmul', pj[:, 0:n], wA[:, kc, mc * 128:(mc + 1) * 128], hT[:, kc, s0:s0 + n],
                                start=(kc == 0), stop=(kc == 7), reads=['wsl'] + hTr, writes=[pjn])
                        gcol = gqc if mc < 4 else gkc
                        u, un = u_[i2], 'u%d' % i2
                        sq, sqn = sq_[i2], 'sq%d' % i2
                        P.I('act', 'activation', out=u[:, 0:n], in_=pj[:, 0:n], func=AF.Copy, scale=gcol[:, 0:1],
                            reads=[pjn, 'gqc', 'gkc'], writes=[un])
                        P.I('act', 'activation', out=sq[:, 0:n], in_=pj[:, 0:n], func=AF.Square, reads=[pjn], writes=[sqn])

                    def a_p2(s0, n, mc, i2, ib=None):
                        i2 = i2 if ib is None else ib
                        pss, pssn = banks[2]
                        ppu, ppun = banks[3]
                        isq = mc < 4
                        dest = qT[:, mc, s0 - C:s0 - C + n] if isq else kT[:, mc - 4, s0:s0 + n]
                        destn = 'qT' if isq else 'kT'
                        u, un = u_[i2], 'u%d' % i2
                        sq, sqn = sq_[i2], 'sq%d' % i2
                        rr, rrn = rr_[i2], 'rr%d' % i2
                        t1, t1n = t1_[i2], 't1_%d' % i2
                        t2, t2n = t2_[i2], 't2_%d' % i2
                        P.I('pe', 'matmul', pss[:, 0:n], bones_b[:], sq[:, 0:n], start=True, stop=True,
                            reads=['bones_b', sqn], writes=[pssn])
                        P.I('pe', 'matmul', ppu[:, 0:n], perm_b[:], u[:, 0:n], start=True, stop=True,
                            reads=['perm_b', un], writes=[ppun])
                        P.I('act', 'activation', out=lnt[:, 0:n], in_=pss[:, 0:n], func=AF.Ln, scale=1.0 / 64, bias=EPS,
                            reads=[pssn], writes=['lnt'])
                        P.I('act', 'activation', out=rr[:, 0:n], in_=lnt[:, 0:n], func=AF.Exp, scale=-0.5,
                            reads=['lnt'], writes=[rrn])
                        P.I('dve' if OA1 else 'pool', 'tensor_tensor', out=t1[:, 0:n], in0=u[:, 0:n], in1=cos_b[:, s0:s0 + n], op=ALU.mult,
                            reads=[un, 'cos_b'], writes=[t1n])
                        P.I('dve', 'tensor_tensor', out=t2[:, 0:n], in0=ppu[:, 0:n], in1=sin_b[:, s0:s0 + n], op=ALU.mult,
                            reads=[ppun, 'sin_b'], writes=[t2n])
                        P.I('dve', 'tensor_tensor', out=t2[:, 0:n], in0=t2[:, 0:n], in1=t1[:, 0:n], op=ALU.add,
                            reads=[t2n, t1n], writes=[t2n])
                        P.I('dve', 'tensor_tensor', out=dest, in0=t2[:, 0:n], in1=rr[:, 0:n], op=ALU.mult,
                            reads=[t2n, rrn], writes=[destn])

                    def a_vproj(s0, n):
                        for ti in range(s0 // 128, (s0 + n) // 128):
                            bv, bvn = banks[4 + (ti % 2)]
                            for kc in range(8):
                                P.I('pe', 'matmul', bv[:, 0:128], hT[:, kc, ti * 128:(ti + 1) * 128], wA[:, kc, 768:896],
                                    start=(kc == 0), stop=(kc == 7), reads=['wsl', 'hT%d' % ti], writes=[bvn])
                            P.I('act', 'activation', out=vaug[:, ti, :, 0:64], in_=bv[:, 0:128].rearrange("p (h d) -> p h d", d=64),
                                func=AF.Copy, reads=[bvn], writes=['vaug'])

                    aitems = []
                    for (s0, n) in chunks:
                        mcs = ([0, 1, 2, 3] if s0 >= C else []) + [4, 5]
                        for mi_, mc in enumerate(mcs):
                            aitems.append((s0, n, mc, len(aitems) % 2, mi_ == len(mcs) - 1))
                    ALAG = 2 if OAP else 1
                    for i in range(len(aitems) + ALAG):
                        if i < len(aitems):
                            s0, n, mc, i2, lastmc = aitems[i]
                            a_p1(s0, n, mc, i2, ib=(i % 3 if OAP else None))
                            if lastmc:
                                a_vproj(s0, n)
                        if i >= ALAG:
                            s0, n, mc, i2, lastmc = aitems[i - ALAG]
                            a_p2(s0, n, mc, i2, ib=((i - ALAG) % 3 if OAP else None))
                    if b == 0:
                        dump("qT", qT[:], [128, 4, T], BF16, ['qT'])
                        dump("kT", kT[:], [128, 2, S], BF16, ['kT'])
                        dump("vaug", vaug[:], [128, 18, 2, 66], BF16, ['vaug'])
                    ao_ = [sb(ea, "aox%d" % i, [128, 8, 64], BF16) for i in range(2)]
                    pvA, pvAn = banks[4]
                    pvB, pvBn = banks[5]

                    def kbs_of(n_):
                        kbs = [(0, None), (1, None)]
                        if n_ > 0:
                            kbs.append((n_ + 1, 0))
                        kbs.append((n_ + 2, None))
                        if n_ < 15:
                            kbs.append((n_ + 3, 1))
                        return kbs

                    def emit_scores(n_, hq, pti=None):
                        kbs = kbs_of(n_)
                        nk = len(kbs)
                        kvh = hq // 4
                        ch = hq // 2
                        par = hq % 2
                        rows = slice(par * 64, par * 64 + 64)
                        sc = PP[hq % 2]
                        scn = ['PP%da' % (hq % 2), 'PP%db' % (hq % 2)]
                        pti = (hq % 2) if pti is None else pti
                        pt = pt_[pti]
                        ptn = 'pt%d' % pti
                        for j, (kt, mi) in enumerate(kbs):
                            P.I('pe', 'matmul', sc[:, j * 128:(j + 1) * 128], kT[rows, kvh, kt * 128:(kt + 1) * 128],
                                qT[rows, ch, n_ * 128:(n_ + 1) * 128], start=True, stop=True,
                                reads=['kT', 'qT'], writes=scn)
                        P.I('act', 'activation', out=pt[:, 0:nk * 128], in_=sc[:, 0:nk * 128], func=AF.Exp,
                            reads=scn, writes=[ptn])
                        for j, (kt, mi) in enumerate(kbs):
                            if mi is not None:
                                P.I('pool' if (OB5 and mi == 1) else 'dve', 'tensor_tensor', out=pt[:, j * 128:(j + 1) * 128], in0=pt[:, j * 128:(j + 1) * 128],
                                    in1=mattn[:, mi, :], op=ALU.mult, reads=[ptn, 'mattn'], writes=[ptn])

                    def emit_pv(n_, hq, pti=None):
                        kbs = kbs_of(n_)
                        nk = len(kbs)
                        kvh = hq // 4
                        pti = (hq % 2) if pti is None else pti
                        pt = pt_[pti]
                        ptn = 'pt%d' % pti
                        pv = (pvA if hq < 4 else pvB)[:, 0:264].rearrange("p (h d) -> p h d", d=66)
                        pvn = pvAn if hq < 4 else pvBn
                        for j, (kt, mi) in enumerate(kbs):
                            P.I('pe', 'matmul', pv[:, hq % 4, 0:65], pt[:, j * 128:(j + 1) * 128], vaug[:, kt, kvh, 0:65],
                                start=(j == 0), stop=(j == nk - 1), reads=[ptn, 'vaug'], writes=[pvn])

                    def emit_fin(n_):
                        ao = ao_[n_ % 2]
                        aon = 'aox%d' % (n_ % 2)
                        for hb, (pvx, pvxn) in enumerate(((pvA, pvAn), (pvB, pvBn))):
                            pv = pvx[:, 0:264].rearrange("p (h d) -> p h d", d=66)
                            P.I('dve', 'tensor_tensor', out=den[:, hb * 4:(hb + 1) * 4, :], in0=pv[:, :, 64:65],
                                in1=es_t[:, hb * 4:(hb + 1) * 4, :], op=ALU.add, reads=[pvxn, 'es_t'], writes=['den'])
                        P.I('dve', 'reciprocal', out=rec[:], in_=den[:], reads=['den'], writes=['rec'])
                        for hb, (pvx, pvxn) in enumerate(((pvA, pvAn), (pvB, pvBn))):
                            pv = pvx[:, 0:264].rearrange("p (h d) -> p h d", d=66)
                            P.I('dve', 'tensor_tensor', out=ao[:, hb * 4:(hb + 1) * 4, :], in0=pv[:, :, 0:64],
                                in1=rec[:, hb * 4:(hb + 1) * 4, :].to_broadcast([128, 4, 64]), op=ALU.mult,
                                reads=[pvxn, 'rec'], writes=[aon])

                    def emit_tr(n_):
                        ao = ao_[n_ % 2]
                        aon = 'aox%d' % (n_ % 2)
                        trn = 'BK%d' % (2 + n_ % 2)
                        trb = BKb[2 + n_ % 2][:, 0:512].rearrange("p (c t) -> p c t", t=128)
                        ao2 = ao[:].rearrange("p h d -> p (h d)")
                        for c4 in range(4):
                            P.I('pe', 'transpose', trb[:, c4, :], ao2[:, c4 * 128:(c4 + 1) * 128], ident_b[:],
                                reads=[aon, 'ident_b'], writes=[trn])
                        P.I('act', 'activation', out=mixT[:, 0:4, n_ * 128:(n_ + 1) * 128], in_=trb, func=AF.Copy,
                            reads=[trn], writes=['mixT_a%d' % n_])

                    items = [(n_, hq) for n_ in range(16) for hq in range(8)]
                    pend_tr = []
                    LAG = max(OATT, 2) if OATT else 1
                    NPT = LAG + 1
                    for i in range(len(items) + LAG):
                        if i < len(items):
                            emit_scores(*items[i], pti=(i % NPT if OATT else None))
                        if i >= LAG:
                            n_, hq = items[i - LAG]
                            emit_pv(n_, hq, pti=((i - LAG) % NPT if OATT else None))
                            if hq == 7:
                                emit_fin(n_)
                                pend_tr.append((i + 2, n_))
                        while pend_tr and (pend_tr[0][0] <= i or i == len(items) + LAG - 1):
                            emit_tr(pend_tr.pop(0)[1])
                    P.end_scope()
                if b == 0:
                    dump("mixT", mixT[:], [128, 8, T], BF16, ['mixT_a%d' % i for i in range(16)])
                if stage <= 2.05:
                    continue

                for pair in range(2):
                    with contextlib.ExitStack() as eb:
                        wB = wsl
                        P.DMA('pool', wB[:, :, 0:128], w_in[:, 768 + pair * 128:768 + (pair + 1) * 128].rearrange("(k p) c -> p k c", p=128), writes=['wsl'])
                        P.DMA('pool', wB[:, :, 128:256], w_in[:, 1024 + pair * 128:1024 + (pair + 1) * 128].rearrange("(k p) c -> p k c", p=128), writes=['wsl'])
                        P.DMA('pool', wB[:, :, 256:288], w_in[:, 2304:2336].rearrange("(k p) c -> p k c", p=128), writes=['wsl'])
                        P.DMA('pool', wB[:, :, 288:544], w_in[:, 1280 + pair * 256:1280 + (pair + 1) * 256].rearrange("(k p) c -> p k c", p=128), writes=['wsl'])
                        P.DMA('pool', wB[:, :, 544:800], w_in[:, 1792 + pair * 256:1792 + (pair + 1) * 256].rearrange("(k p) c -> p k c", p=128), writes=['wsl'])
                        qeT = [sb(eb, "qeT%d" % d_, [128, T], BF16) for d_ in range(2)]
                        keT = [sb(eb, "keT%d" % d_, [128, S], BF16) for d_ in range(2)]
                        ketok = [sb(eb, "ketok%d" % d_, [128, 18, 128], BF16) for d_ in range(2)]
                        gv = sb(eb, "gv", [128, 18, 256], BF16)
                        sgg = sb(eb, "sgg", [128, 16, 256], BF16)
                        Sp = [sb(eb, "Sp%d" % d_, [128, 32, 128], BF16) for d_ in range(2)]
                        a_all = [sb(eb, "a_all%d" % d_, [128, 36], F32) for d_ in range(2)]
                        b_all = [sb(eb, "b_all%d" % d_, [128, 36], F32) for d_ in range(2)]
                        er_all = [sb(eb, "er_all%d" % d_, [128, 36], F32) for d_ in range(2)]
                        lrT = sb(eb, "lrT", [32, 512], BF16)
                        e_t2 = [sb(eb, "e_t%d" % i, [128, 512], F32) for i in range(2)]
                        cs_t2 = [sb(eb, "cs_t%d" % i, [128, 512], F32) for i in range(2)]
                        sf_t = sb(eb, "sf_t", [128, 512], F32)
                        dl_t2 = [sb(eb, "dl_t%d" % i, [128, 512], F32) for i in range(2)]
                        eq_t2 = [sb(eb, "eq_t%d" % i, [128, 512], BF16) for i in range(2)]
                        ek_t2 = [sb(eb, "ek_t%d" % i, [128, 512], BF16) for i in range(2)]
                        sgt = sb(eb, "sgt", [128, 256], F32)
                        Sst = [[sb(eb, "Sst%d_%d" % (d_, i), [128, 128], F32) for i in range(3)] for d_ in range(2)]
                        kvs = [sb(eb, "kvs%d" % i, [128, 128], F32) for i in range(4)]
                        asb = sb(eb, "asb", [128, 2, 2, 128], BF16)
                        ssq = sb(eb, "ssq", [128, 4], F32)
                        ojk = sb(eb, "ojk", [128, 128], BF16)
                        og1 = sb(eb, "og1", [128, 2, 128], F32)
                        og2 = sb(eb, "og2", [128, 256], BF16)
                        def ke_transposes(tls):
                            for ti in tls:
                                for d_ in range(2):
                                    ktn = 'BK%d' % (2 + d_)
                                    ktp = BKb[2 + d_][:, 0:128]
                                    P.I('pe', 'transpose', ktp, keT[d_][:, ti * 128:(ti + 1) * 128], ident_b[:],
                                        reads=['keT%d' % d_, 'ident_b'], writes=[ktn])
                                    (P.I('dve', 'tensor_copy', out=ketok[d_][:, ti, :], in_=ktp, reads=[ktn], writes=['ketok%d' % d_]) if OA4 else P.I('act', 'activation', out=ketok[d_][:, ti, :], in_=ktp, func=AF.Copy, reads=[ktn], writes=['ketok%d' % d_]))

                        prev_tiles = None
                        for (s0, n) in chunks:
                            tiles = list(range(s0 // 128, (s0 + n) // 128))
                            hTr = ['hT%d' % i for i in tiles]
                            nch = n // 64
                            c0 = s0 // 64
                            lat = s0 >= C
                            pgq, pgqn = banks[0]
                            pgk, pgkn = banks[1]
                            plr, plrn = banks[2]
                            for kc in range(8):
                                P.I('pe', 'matmul', plr[0:32, 0:n], wB[:, kc, 256:288], hT[:, kc, s0:s0 + n], start=(kc == 0), stop=(kc == 7),
                                    reads=['wsl'] + hTr, writes=[plrn])
                            P.I('act', 'activation', out=lrT[:, 0:n], in_=plr[0:32, 0:n], func=AF.Copy, reads=[plrn], writes=['lrT'])
                            def qk_proj():
                                if lat:
                                    for kc in range(8):
                                        P.I('pe', 'matmul', pgq[:, 0:n], wB[:, kc, 0:128], hT[:, kc, s0:s0 + n], start=(kc == 0), stop=(kc == 7),
                                            reads=['wsl'] + hTr, writes=[pgqn])
                                for kc in range(8):
                                    P.I('pe', 'matmul', pgk[:, 0:n], wB[:, kc, 128:256], hT[:, kc, s0:s0 + n], start=(kc == 0), stop=(kc == 7),
                                        reads=['wsl'] + hTr, writes=[pgkn])
                            cs3s, Ccs, ris, tis, dl3s = {}, {}, {}, {}, {}

                            def s1(d_):
                                pz, pzn = banks[3 - d_]
                                e_ = e_t2[d_]
                                P.I('pe', 'matmul', pz[:, 0:n], wdx[:, d_, pair * 128:(pair + 1) * 128], lrT[:, 0:n], start=True, stop=True,
                                    reads=['wdx', 'lrT'], writes=[pzn])
                                P.I('act', 'activation', out=e_[:, 0:n], in_=pz[:, 0:n], func=AF.Exp, scale=-1.0,
                                    bias=nbd[:, d_, pair:pair + 1], reads=[pzn, 'nbd'], writes=['e_t%d' % d_])
                                P.I('act', 'activation', out=e_[:, 0:n], in_=e_[:, 0:n], func=AF.Ln, bias=1.0,
                                    reads=['e_t%d' % d_], writes=['e_t%d' % d_])

                            def s2(d_):
                                e_ = e_t2[d_]
                                cs_ = cs_t2[d_]
                                P.I('dve', 'tensor_tensor_scan', out=cs_[:, 0:n], data0=rmask[:, 0:n], data1=e_[:, 0:n], initial=0.0,
                                    op0=ALU.mult, op1=ALU.add, reads=['rmask', 'e_t%d' % d_], writes=['cs_t%d' % d_])
                                cs3 = cs_[:, 0:n].rearrange("p (c i) -> p c i", i=64)
                                if d_ == 0:
                                    Cc, Ccn = cs3, 'cs_t0'
                                    ri, ti_ = 31, 63
                                else:
                                    sf3 = sf_t[:, 0:n].rearrange("p (c i) -> p c i", i=64)
                                    P.I('dve', 'tensor_tensor', out=sf_t[:, 0:n], in0=e_[:, 0:n], in1=cs_[:, 0:n], op=ALU.subtract,
                                        reads=['e_t1', 'cs_t1'], writes=['sf_t'])
                                    P.I('dve', 'tensor_tensor', out=sf3, in0=sf3, in1=cs3[:, :, 63:64].to_broadcast([128, nch, 64]),
                                        op=ALU.add, reads=['sf_t', 'cs_t1'], writes=['sf_t'])
                                    Cc, Ccn = sf3, 'sf_t'
                                    ri, ti_ = 32, 0
                                dl3 = dl_t2[d_][:, 0:n].rearrange("p (c i) -> p c i", i=64)
                                P.I('dve', 'tensor_tensor', out=dl3, in0=Cc, in1=Cc[:, :, ri:ri + 1].to_broadcast([128, nch, 64]),
                                    op=ALU.subtract, reads=[Ccn], writes=['dl_t%d' % d_])
                                Ccs[d_], ris[d_], tis[d_], dl3s[d_] = (Cc, Ccn), ri, ti_, dl3

                            def s3(d_):
                                (Cc, Ccn), ri, ti_, dl3 = Ccs[d_], ris[d_], tis[d_], dl3s[d_]
                                dln = 'dl_t%d' % d_
                                P.I('act', 'activation', out=ek_t2[d_][:, 0:n], in_=dl_t2[d_][:, 0:n], func=AF.Exp, scale=1.0 / 16,
                                    reads=[dln], writes=['ek_t%d' % d_])
                                if lat:
                                    P.I('act', 'activation', out=eq_t2[d_][:, 0:n], in_=dl_t2[d_][:, 0:n], func=AF.Exp, scale=-1.0 / 16,
                                        reads=[dln], writes=['eq_t%d' % d_])
                                P.I('act', 'activation', out=a_all[d_][:, c0:c0 + nch].unsqueeze(2), in_=Cc[:, :, ti_:ti_ + 1], func=AF.Exp,
                                    scale=-1.0 / 16, reads=[Ccn], writes=['a_all%d' % d_])
                                P.I('act', 'activation', out=er_all[d_][:, c0:c0 + nch].unsqueeze(2), in_=Cc[:, :, ri:ri + 1], func=AF.Exp,
                                    scale=-1.0 / 16, reads=[Ccn], writes=['er_all%d' % d_])
                                P.I('act', 'activation', out=b_all[d_][:, c0:c0 + nch].unsqueeze(2), in_=dl3[:, :, ti_:ti_ + 1], func=AF.Exp,
                                    scale=-1.0 / 16, reads=[dln], writes=['b_all%d' % d_])

                            def s4(d_):
                                if lat:
                                    P.I('dve', 'scalar_tensor_tensor', out=qeT[d_][:, s0 - C:s0 - C + n], in0=pgq[:, 0:n], scalar=0.125,
                                        in1=eq_t2[d_][:, 0:n], op0=ALU.mult, op1=ALU.mult, reads=[pgqn, 'eq_t%d' % d_], writes=['qeT%d' % d_])
                                P.I('dve', 'tensor_tensor', out=keT[d_][:, s0:s0 + n], in0=pgk[:, 0:n], in1=ek_t2[d_][:, 0:n], op=ALU.mult,
                                    reads=[pgkn, 'ek_t%d' % d_], writes=['keT%d' % d_])

                            if OPT_REORD:
                                for d_ in range(2):
                                    s1(d_)
                                if prev_tiles:
                                    ke_transposes(prev_tiles)
                                qk_proj()
                            else:
                                qk_proj()
                                for d_ in range(2):
                                    s1(d_)
                            for stg in (s2, s3, s4):
                                for d_ in range(2):
                                    stg(d_)
                            prev_tiles = tiles
                            for ti in (tiles if BIS >= 5 else []):
                                pgv, pgvn = banks[4 + (ti % 2)]
                                for kc in range(8):
                                    P.I('pe', 'matmul', pgv[:, 0:256], hT[:, kc, ti * 128:(ti + 1) * 128], wB[:, kc, 288:544],
                                        start=(kc == 0), stop=(kc == 7), reads=['wsl', 'hT%d' % ti], writes=[pgvn])
                                (P.I('dve', 'tensor_copy', out=gv[:, ti, :], in_=pgv[:, 0:256], reads=[pgvn], writes=['gv']) if OB1 else P.I('act', 'activation', out=gv[:, ti, :], in_=pgv[:, 0:256], func=AF.Copy, reads=[pgvn], writes=['gv']))
                                if lat:
                                    for kc in range(8):
                                        P.I('pe', 'matmul', pgv[:, 256:512], hT[:, kc, ti * 128:(ti + 1) * 128], wB[:, kc, 544:800],
                                            start=(kc == 0), stop=(kc == 7), reads=['wsl', 'hT%d' % ti], writes=[pgvn])
                                    P.I('act', 'activation', out=sgt[:], in_=pgv[:, 256:512], func=AF.Silu, reads=[pgvn], writes=['sgt'])
                                    P.I('pool' if OA6 else 'dve', 'tensor_tensor', out=sgg[:, ti - 2, :].rearrange("p (h d) -> p h d", d=128),
                                        in0=sgt[:].rearrange("p (h d) -> p h d", d=128),
                                        in1=glab[:].unsqueeze(1).to_broadcast([128, 2, 128]), op=ALU.mult,
                                        reads=['sgt', 'glab'], writes=['sgg'])
                            if not OPT_REORD:
                                ke_transposes(tiles)
                        if OPT_REORD:
                            ke_transposes(prev_tiles)
                        if pair == 1 and stage > 3 and OPT_CPREP:
                            c_prep(b)
                        orders = [list(range(36)), [3, 2, 1, 0] + list(range(35, 3, -1))]
                        for d_ in range(2):
                            P.I('pool', 'memset', Sst[d_][0][:], 0.0, writes=['Sst%d_0' % d_])
                        kvi = 0
                        for step in range(36 if stage >= 2.4 else 0):
                            for d_ in range(2):
                                n_ = orders[d_][step]
                                Scur, Scn = Sst[d_][step % 3], 'Sst%d_%d' % (d_, step % 3)
                                Snx, Snn = Sst[d_][(step + 1) % 3], 'Sst%d_%d' % (d_, (step + 1) % 3)
                                if n_ >= 4:
                                    if OB2:
                                        P.I('pool', 'tensor_scalar', out=Sp[d_][:, n_ - 4, :], in0=Scur[:], scalar1=er_all[d_][:, n_:n_ + 1], scalar2=None,
                                            op0=ALU.mult, reads=[Scn, 'er_all%d' % d_], writes=['Sp%d' % d_])
                                    else:
                                        P.I('act', 'activation', out=Sp[d_][:, n_ - 4, :], in_=Scur[:], func=AF.Copy,
                                            scale=er_all[d_][:, n_:n_ + 1], reads=[Scn, 'er_all%d' % d_], writes=['Sp%d' % d_])
                                if step == 35:
                                    continue
                                ti = n_ // 2
                                cp = n_ % 2
                                kvp, kvpn = banks[6 + (kvi % 2)]
                                kv_, kvn = kvs[kvi % 4], 'kvs%d' % (kvi % 4)
                                kvi += 1
                                for hh in range(2):
                                    P.I('pe', 'matmul', kvp[hh * 64:(hh + 1) * 64, 0:128],
                                        ketok[d_][cp * 64:(cp + 1) * 64, ti, hh * 64:(hh + 1) * 64],
                                        gv[cp * 64:(cp + 1) * 64, ti, hh * 128:(hh + 1) * 128], start=True, stop=True,
                                        tile_position=(cp * 64, hh * 64), reads=['ketok%d' % d_, 'gv'], writes=[kvpn])
                                if d_ == 0 or not OPT_REC:
                                    if OB3:
                                        P.I('act', 'activation', out=kv_[:], in_=kvp[:, 0:128], func=AF.Copy, scale=b_all[d_][:, n_:n_ + 1],
                                            reads=[kvpn, 'b_all%d' % d_], writes=[kvn])
                                    else:
                                        P.I('dve', 'tensor_scalar', out=kv_[:], in0=kvp[:, 0:128], scalar1=b_all[d_][:, n_:n_ + 1], scalar2=None,
                                            op0=ALU.mult, reads=[kvpn, 'b_all%d' % d_], writes=[kvn])
                                    P.I('dve', 'scalar_tensor_tensor', out=Snx[:], in0=Scur[:], scalar=a_all[d_][:, n_:n_ + 1], in1=kv_[:],
                                        op0=ALU.mult, op1=ALU.add, reads=[Scn, kvn, 'a_all%d' % d_], writes=[Snn])
                                else:
                                    P.I('act', 'activation', out=kv_[:], in_=kvp[:, 0:128], func=AF.Copy, scale=b_all[d_][:, n_:n_ + 1],
                                        reads=[kvpn, 'b_all%d' % d_], writes=[kvn])
                                    P.I('pool', 'tensor_scalar', out=Snx[:], in0=Scur[:], scalar1=a_all[d_][:, n_:n_ + 1], scalar2=None,
                                        op0=ALU.mult, reads=[Scn, 'a_all%d' % d_], writes=[Snn])
                                    P.I('pool', 'tensor_tensor', out=Snx[:], in0=Snx[:], in1=kv_[:], op=ALU.add,
                                        reads=[Snn, kvn], writes=[Snn])
                        asb2 = [asb, sb(eb, "asbb", [128, 2, 2, 128], BF16)] + ([sb(eb, "asbc", [128, 2, 2, 128], BF16)] if OG2 else [])
                        NAS = len(asb2)
                        og2b = [og2, sb(eb, "og2b", [128, 256], BF16)]

                        def g_partA(tt):
                            st_ = tt + 2
                            apn = ['PP0a', 'PP0b']
                            aps = PP[0][:, :].rearrange("p (h x) -> p h x", h=2)[:, :, 0:256].rearrange("p h (d i) -> p h d i", d=2)
                            for hh in range(2):
                                rows = slice(hh * 64, hh * 64 + 64)
                                for d_ in range(2):
                                    P.I('pe', 'matmul', aps[:, hh, d_, :], keT[d_][rows, st_ * 128:(st_ + 1) * 128],
                                        qeT[d_][rows, tt * 128:(tt + 1) * 128], start=True, stop=True,
                                        reads=['keT%d' % d_, 'qeT%d' % d_], writes=[apn[hh]])
                            P.I('dve', 'tensor_tensor', out=asb2[tt % NAS][:], in0=aps, in1=mgla[:].unsqueeze(1).to_broadcast([128, 2, 2, 128]),
                                op=ALU.mult, reads=apn + ['mgla'], writes=['asb%d' % (tt % NAS)])

                        def g_partB(tt):
                            st_ = tt + 2
                            asb_ = asb2[tt % NAS]
                            asn = 'asb%d' % (tt % NAS)
                            opn = ['PP1a', 'PP1b']
                            ops_ = PP[1][:, :].rearrange("p (h x) -> p h x", h=2)[:, :, 0:128]
                            for hh in range(2):
                                rows = slice(hh * 64, hh * 64 + 64)
                                for d_ in range(2):
                                    P.I('pe', 'matmul', ops_[:, hh, :], asb_[:, hh, d_, :], gv[:, st_, hh * 128:(hh + 1) * 128],
                                        start=(d_ == 0), stop=False, reads=[asn, 'gv'], writes=[opn[hh]])
                                for cp in range(2):
                                    ch_ = tt * 2 + cp
                                    for d_ in range(2):
                                        last = (d_ == 1)
                                        P.I('pe', 'matmul', ops_[cp * 64:(cp + 1) * 64, hh, :],
                                            qeT[d_][rows, ch_ * 64:(ch_ + 1) * 64], Sp[d_][rows, ch_, :],
                                            start=False, stop=last, tile_position=(hh * 64, cp * 64),
                                            reads=['qeT%d' % d_, 'Sp%d' % d_], writes=[opn[hh]])
                            for hh in range(2):
                                P.I('act', 'activation', out=ojk[:], in_=ops_[:, hh, :], func=AF.Square, accum_out=ssq[:, hh:hh + 1],
                                    reads=[opn[hh]], writes=['ojk', 'ssq'])
                            P.I('act', 'activation', out=ssq[:, 2:4], in_=ssq[:, 0:2], func=AF.Ln, scale=1.0 / 128, bias=EPS,
                                reads=['ssq'], writes=['ssq'])
                            P.I('act', 'activation', out=ssq[:, 0:2], in_=ssq[:, 2:4], func=AF.Exp, scale=-0.5, reads=['ssq'], writes=['ssq'])
                            P.I('dve', 'tensor_tensor', out=og1[:], in0=ops_, in1=ssq[:, 0:2].unsqueeze(2).to_broadcast([128, 2, 128]),
                                op=ALU.mult, reads=opn + ['ssq'], writes=['og1'])
                            P.I('pool' if OB4 else 'dve', 'tensor_tensor', out=og2b[tt % 2][:], in0=og1[:].rearrange("p h v -> p (h v)"), in1=sgg[:, tt, :], op=ALU.mult,
                                reads=['og1', 'sgg'], writes=['og2_%d' % (tt % 2)])

                        def g_partC(tt):
                            trn = 'BK%d' % (tt % 2)
                            trb = BKb[tt % 2][:, 0:256].rearrange("p (c t) -> p c t", t=128)
                            for c2 in range(2):
                                P.I('pe', 'transpose', trb[:, c2, :], og2b[tt % 2][:, c2 * 128:(c2 + 1) * 128], ident_b[:],
                                    reads=['og2_%d' % (tt % 2), 'ident_b'], writes=[trn])
                            P.I('act', 'activation', out=mixT[:, 4 + 2 * pair:6 + 2 * pair, tt * 128:(tt + 1) * 128], in_=trb, func=AF.Copy,
                                reads=[trn], writes=['mixT_g%d_%d' % (pair, tt)])

                        NTT = 16 if stage >= 2.7 else 0
                        GL = 2 if OG2 else 1
                        for i in range(NTT + GL + 1):
                            if i < NTT:
                                g_partA(i)
                            if GL <= i < NTT + GL:
                                g_partB(i - GL)
                            if GL + 1 <= i:
                                g_partC(i - GL - 1)
                        P.end_scope()
                if b == 0:
                    dump("mixT2", mixT[:], [128, 8, T], BF16, ['mixT_g1_%d' % i for i in range(16)])
                if stage <= 3:
                    continue

                with contextlib.ExitStack() as ec:
                    wO = wsl
                    sc2bc = sb(ec, "sc2bc", [128, D], F32)
                    sh2bc = sb(ec, "sh2bc", [128, D], F32)
                    n2gb = sb(ec, "n2gb", [128, D], F32)
                    P.DMA('sp', n2gb[:], n2g.partition_broadcast(128), writes=['n2gb'])
                    if not OPT_CPREP:
                        c_prep(b)
                    bcast_row(sc2bc, 'sc2bc', 32, b)
                    bcast_row(sh2bc, 'sh2bc', 24, b)
                    P.I('dve', 'tensor_scalar', out=sc2bc[:], in0=sc2bc[:], scalar1=1.0, scalar2=None, op0=ALU.add,
                        reads=['sc2bc'], writes=['sc2bc'])
                    P.I('dve', 'tensor_tensor', out=sc2bc[:], in0=sc2bc[:], in1=n2gb[:], op=ALU.mult, reads=['sc2bc', 'n2gb'], writes=['sc2bc'])
                    xr = [sb(ec, "xr%d" % i, [128, D], F32) for i in range(3 + OCL)]
                    x1t = [sb(ec, "x1t%d" % i, [128, D], F32) for i in range(3 + OCL)]
                    h2t = [sb(ec, "h2t%d" % i, [128, D], F32) for i in range(3 + OCL)]
                    h2Tb = [sb(ec, "h2T%d" % i, [128, 8, 128], F32) for i in range(2 if OC8 else 1)]
                    sqj2 = sb(ec, "sqj2", [128, D], BF16)
                    s4 = [sb(ec, "s4_%d" % i, [128, 8], F32) for i in range(3 + OCL)]
                    ex = sb(ec, "ex", [128, NE], F32)
                    lgs = sb(ec, "lgs", [NE, 128], F32)

                    def c_partA(tt):
                        i2 = tt % (3 + OCL)
                        xr_, xrn = xr[i2], 'xr%d' % i2
                        x1_, x1n = x1t[i2], 'x1t%d' % i2
                        h2_, h2n = h2t[i2], 'h2t%d' % i2
                        s4_, s4n = s4[i2], 's4_%d' % i2
                        P.DMA('sp', xr_[:], x[b, tt * 128:(tt + 1) * 128, :], writes=[xrn])
                        for half in range(2):
                            po, pon = banks[half]
                            for kc in range(8):
                                P.I('pe', 'matmul', po, mixT[:, kc, tt * 128:(tt + 1) * 128], wO[:, kc, half * 512:(half + 1) * 512],
                                    start=(kc == 0), stop=(kc == 7),
                                    reads=['wsl', 'mixT_a%d' % tt, 'mixT_g0_%d' % tt, 'mixT_g1_%d' % tt], writes=[pon])
                            P.I('dve', 'tensor_tensor', out=x1_[:, half * 512:(half + 1) * 512], in0=po, in1=xr_[:, half * 512:(half + 1) * 512],
                                op=ALU.add, reads=[pon, xrn], writes=[x1n])
                        P.DMA('sp', out[b, tt * 128:(tt + 1) * 128, :], x1_[:], reads=[x1n], writes=['out_x1'])
                        P.I('act', 'activation', out=sqj2[:], in_=x1_[:], func=AF.Square, accum_out=s4_[:, 0:1], reads=[x1n], writes=['sqj2', s4n])
                        P.I('act', 'activation', out=s4_[:, 1:2], in_=s4_[:, 0:1], func=AF.Ln, scale=1.0 / D, bias=EPS, reads=[s4n], writes=[s4n])
                        P.I('act', 'activation', out=s4_[:, 2:3], in_=s4_[:, 1:2], func=AF.Exp, scale=-0.5, reads=[s4n], writes=[s4n])
                        if OC7:
                            return
                        c_partA2(tt)

                    def c_partA2(tt):
                        i2 = tt % (3 + OCL)
                        x1_, x1n = x1t[i2], 'x1t%d' % i2
                        h2_, h2n = h2t[i2], 'h2t%d' % i2
                        s4_, s4n = s4[i2], 's4_%d' % i2
                        P.I('dve', 'scalar_tensor_tensor', out=h2_[:], in0=x1_[:], scalar=s4_[:, 2:3], in1=sc2bc[:], op0=ALU.mult, op1=ALU.mult,
                            reads=[x1n, s4n, 'sc2bc'], writes=[h2n])
                        P.I('dve' if OA5 else 'pool', 'tensor_tensor', out=h2_[:], in0=h2_[:], in1=sh2bc[:], op=ALU.add, reads=[h2n, 'sh2bc'], writes=[h2n])
                        P.DMA('pool', h2d[b * T + tt * 128:b * T + (tt + 1) * 128, :], h2_[:], reads=[h2n], writes=['h2d'])

                    def c_partB(tt):
                        i2 = tt % (3 + OCL)
                        h2_, h2n = h2t[i2], 'h2t%d' % i2
                        s4_, s4n = s4[i2], 's4_%d' % i2
                        for half in range(2):
                            trf, trfn = banks[2 + half]
                            for q4 in range(4):
                                kc = half * 4 + q4
                                P.I('pe', 'transpose', trf[:, q4 * 128:(q4 + 1) * 128], h2_[:, kc * 128:(kc + 1) * 128], ident_f[:],
                                    reads=[h2n, 'ident_f'], writes=[trfn])
                            h2T = h2Tb[tt % 2] if OC8 else h2Tb[0]
                            P.I('dve', 'tensor_copy', out=h2T[:, half * 4:(half + 1) * 4, :].rearrange("p k t -> p (k t)"), in_=trf,
                                reads=[trfn], writes=['h2T%d' % (tt % 2 if OC8 else 0)])
                        if OC8:
                            return
                        c_partB2(tt)

                    def c_partB2(tt):
                        i2 = tt % (3 + OCL)
                        s4_, s4n = s4[i2], 's4_%d' % i2
                        h2T = h2Tb[tt % 2] if OC8 else h2Tb[0]
                        h2Tn = 'h2T%d' % (tt % 2 if OC8 else 0)
                        lg, lgn = banks[4 + tt % 2]
                        for kc in range(8):
                            P.I('pe', 'matmul', lg[0:NE, 128:256], wr[:, kc, :], h2T[:, kc, :], start=(kc == 0), stop=(kc == 7),
                                reads=[h2Tn, 'wr'], writes=[lgn])
                        P.I('act', 'activation', out=lgs[:], in_=lg[0:NE, 128:256], func=AF.Copy, reads=[lgn], writes=['lgs'])
                        P.I('pe', 'transpose', lg[:, 0:NE], lgs[:], ident_f[0:NE, 0:NE], reads=['lgs', 'ident_f'], writes=[lgn])
                        P.I('dve', 'tensor_reduce', out=s4_[:, 3:4], in_=lg[:, 0:NE], axis=AX.X, op=ALU.max, negate=True, reads=[lgn], writes=[s4n])
                        P.I('act', 'activation', out=ex[:], in_=lg[:, 0:NE], func=AF.Exp, bias=s4_[:, 3:4], accum_out=s4_[:, 4:5],
                            reads=[lgn, s4n], writes=['ex', s4n])
                        P.I('dve', 'reciprocal', out=s4_[:, 5:6], in_=s4_[:, 4:5], reads=[s4n], writes=[s4n])
                        P.I('dve', 'tensor_scalar', out=aff[:, b, tt, :], in0=ex[:], scalar1=s4_[:, 5:6], scalar2=None, op0=ALU.mult,
                            reads=['ex', s4n], writes=['aff'])

                    for tt in range(21):
                        if tt < 16:
                            c_partA(tt)
                        if 2 + OCL <= tt < 18 + OCL:
                            c_partB(tt - 2 - OCL)
                        if OC8 and 3 + OCL <= tt < 19 + OCL:
                            c_partB2(tt - 3 - OCL)
                        if OC7 and not OC9 and tt < 16:
                            c_partA2(tt)
                        if OC7 and OC9 and 1 <= tt < 17:
                            c_partA2(tt - 1)
                    P.DMA('sp', affd[b * T:(b + 1) * T, :].rearrange("(t p) e -> p t e", p=128), aff[:, b, :, :], reads=['aff'], writes=['affd'])
                    atmp = sb(ec, "atmp", [NE, T], F32)
                    for g4 in range(4):
                        tb, tbn = banks[6 + (g4 % 2)]
                        for q4 in range(4):
                            tt = g4 * 4 + q4
                            P.I('pe', 'transpose', tb[0:NE, q4 * 128:(q4 + 1) * 128], aff[:, b, tt, :], ident_f[:],
                                reads=['aff', 'ident_f'], writes=[tbn])
                        P.I('act', 'activation', out=atmp[:, g4 * 512:(g4 + 1) * 512], in_=tb[0:NE, :], func=AF.Copy, reads=[tbn], writes=['atmp'])
                    P.DMA('sp', affT[b * NE:(b + 1) * NE, :], atmp[:], reads=['atmp'], writes=['affT'])
                    P.end_scope()
            dump("aff", aff[:], [128, NB, 16, NE], F32, ['aff'])
            dump("affT", affT[:], [R, T], F32, ['affT'])
            P.barrier()

        if stage >= 5:
            idx_i = sb(es, "idx_i", [128, R, 2], I32)
            g2bc = sb(es, "g2bc", [128, NB, D], F32)
            wg = [sb(es, "wg%d" % i, [128, 8, D], BF16) for i in range(2)]
            wu = [sb(es, "wu%d" % i, [128, 8, D], BF16) for i in range(2)]
            wd = [sb(es, "wd%d" % i, [128, 8, D], BF16) for i in range(2)]

            def load_w(e):
                i2 = e % 2
                for (dst, src, nm) in ((wg[i2], weg, 'wg%d' % i2), (wu[i2], weu, 'wu%d' % i2), (wd[i2], wed, 'wd%d' % i2)):
                    for k2 in range(4):
                        P.DMA('poolw', dst[:, 2 * k2:2 * k2 + 2, :], src[e, k2 * 256:(k2 + 1) * 256, :].rearrange("(k p) f -> p k f", p=128),
                              writes=['%s_k%d' % (nm, k2)])


            load_w(0)
            load_w(1)
            with contextlib.ExitStack() as er:
                rep2 = [sb(er, "repb%d" % i, [128, 128], F32) for i in range(2)]
                for b in range(NB):
                    for half in range(2):
                        bk, bkn = banks[6 + half]
                        for q4 in range(4):
                            kc = half * 4 + q4
                            r_ = rep2[q4 % 2]
                            rn = 'repb%d' % (q4 % 2)
                            P.I('dve', 'tensor_copy', out=r_[:], in_=modT[:, 40 + kc, b:b + 1].to_broadcast([128, 128]), reads=['modT'], writes=[rn])
                            P.I('pe', 'matmul', bk[:, q4 * 128:(q4 + 1) * 128], r_[:], ident_f[:], start=True, stop=True,
                                reads=[rn, 'ident_f'], writes=[bkn])
                        P.I('act', 'activation', out=g2bc[:, b, half * 512:(half + 1) * 512], in_=bk, func=AF.Copy, reads=[bkn], writes=['g2bc'])
                lo = sb(er, "lo", [R, 1], F32)
                hi = sb(er, "hi", [R, 1], F32)
                mid = sb(er, "mid", [R, 1], F32)
                cntt = sb(er, "cntt", [R, 1], F32)
                sel = sb(er, "sel", [R, 1], F32)
                tmpb = sb(er, "tmpb", [R, 1], F32)
                junk = sb(er, "junk", [128, T], BF16)
                mask = sb(er, "mask", [R, T], F32)
                cum = sb(er, "cum", [R, T], F32)
                zer = sb(er, "zer", [R, T], F32)
                P.I('pool', 'memset', zer[:], 0.0, writes=['zer'])
                slot = sb(er, "slot", [128, 2], F32)
                idxf = sb(er, "idxf", [128, R, 2], F32)
                NCB = 3 if OPT_I16 else 2
                cb = [sb(er, "cb%d" % i, [128, T], I16 if OPT_I16 else F32) for i in range(NCB)]
                junk2 = sb(er, "junk2", [128, T], BF16)
                sloth = sb(er, "sloth", [128, 2], F32)
                P.DMA('sp', slot[:], c_slotid, writes=['slot'])
                P.I('dve', 'tensor_scalar', out=sloth[:], in0=slot[:], scalar1=0.5, scalar2=None, op0=ALU.add, reads=['slot'], writes=['sloth'])
                P.I('dve', 'memset', lo[:], 0.0, writes=['lo'])
                P.I('dve', 'memset', hi[:], 1.0, writes=['hi'])
                for it in range(30):
                    P.I('dve', 'tensor_tensor', out=mid[:], in0=lo[:], in1=hi[:], op=ALU.add, reads=['lo', 'hi'], writes=['mid'])
                    P.I('dve', 'tensor_scalar', out=mid[:], in0=mid[:], scalar1=0.5, scalar2=None, op0=ALU.mult, reads=['mid'], writes=['mid'])
                    P.I('dve', 'tensor_scalar', out=junk[0:R, :], in0=affT[:], scalar1=mid[:, 0:1], scalar2=0.0, op0=ALU.is_ge, op1=ALU.add,
                        accum_out=cntt[:, 0:1], reads=['affT', 'mid'], writes=['junk', 'cntt'])
                    P.I('dve', 'tensor_scalar', out=sel[:], in0=cntt[:], scalar1=float(CAP) - 0.5, scalar2=None, op0=ALU.is_ge,
                        reads=['cntt'], writes=['sel'])
                    P.I('dve', 'scalar_tensor_tensor', out=lo[:], in0=mid[:], scalar=sel[:, 0:1], in1=lo[:], op0=ALU.mult, op1=ALU.max,
                        reads=['mid', 'sel', 'lo'], writes=['lo'])
                    P.I('dve', 'scalar_tensor_tensor', out=tmpb[:], in0=sel[:], scalar=2.0, in1=mid[:], op0=ALU.mult, op1=ALU.add,
                        reads=['mid', 'sel'], writes=['tmpb'])
                    P.I('dve', 'tensor_tensor', out=hi[:], in0=tmpb[:], in1=hi[:], op=ALU.min, reads=['tmpb', 'hi'], writes=['hi'])
                P.I('dve', 'tensor_scalar', out=mask[:], in0=affT[:], scalar1=lo[:, 0:1], scalar2=None, op0=ALU.is_ge,
                    reads=['affT', 'lo'], writes=['mask'])
                P.I('dve', 'tensor_tensor_scan', out=cum[:], data0=zer[:], data1=mask[:], initial=0.0, op0=ALU.add, op1=ALU.add,
                    reads=['mask', 'zer'], writes=['cum'])
                if OPT_RT:
                    ohr = [sb(er, "ohr%d" % i, [R, 128], F32) for i in range(2)]
                    idxp = sb(er, "idxp", [128, R, 2, 4], F32)
                    for r in range(R):
                        oh, ohn = ohr[r % 2], 'ohr%d' % (r % 2)
                        P.I('dve', 'tensor_copy', out=oh[:], in_=ident_f[0:R, r:r + 1].to_broadcast([R, 128]), reads=['ident_f'], writes=[ohn])
                        for c4 in range(4):
                            bk, bkn = banks[(r % 2) * 4 + c4]
                            P.I('pe', 'matmul', bk, oh[:], cum[:, c4 * 512:(c4 + 1) * 512], start=True, stop=True,
                                reads=[ohn, 'cum'], writes=[bkn])
                            for st_ in range(2):
                                if c4 < 2:
                                    P.I('dve', 'tensor_scalar', out=junk[:, 0:512], in0=bk, scalar1=slot[:, st_:st_ + 1], scalar2=0.0, op0=ALU.is_le,
                                        op1=ALU.add, accum_out=idxp[:, r, st_, c4:c4 + 1], reads=[bkn, 'slot'], writes=['junk', 'idxp'])
                                else:
                                    P.I('act', 'activation', out=junk2[:, 0:512], in_=bk, func=AF.Sign, scale=-1.0, bias=sloth[:, st_:st_ + 1],
                                        accum_out=idxp[:, r, st_, c4:c4 + 1], reads=[bkn, 'sloth'], writes=['junk2', 'idxp_a'])
                    P.I('dve', 'tensor_scalar', out=idxp[:, :, :, 2:4], in0=idxp[:, :, :, 2:4], scalar1=0.5, scalar2=256.0, op0=ALU.mult, op1=ALU.add,
                        reads=['idxp', 'idxp_a'], writes=['idxp', 'idxp_a'])
                    P.I('dve', 'tensor_reduce', out=idxf[:].rearrange("p r s -> p (r s)"), in_=idxp[:].rearrange("p r s c -> p (r s) c"),
                        axis=AX.X, op=ALU.add, reads=['idxp', 'idxp_a'], writes=['idxf', 'idxf_a'])
                else:
                    if OPT_I16:
                        cum16 = sb(er, "cum16", [R, T], I16)
                        P.I('dve', 'tensor_copy', out=cum16[:], in_=cum[:], reads=['cum'], writes=['cum16'])
                        P.DMA('sp', cumd, cum16[:], reads=['cum16'], writes=['cumd'])
                    else:
                        P.DMA('sp', cumd, cum[:], reads=['cum'], writes=['cumd'])
                    for r in range(R):
                        b = r // NE
                        cb_, cbn = cb[r % NCB], 'cb%d' % (r % NCB)
                        P.DMA('sp', cb_[:], cumd[r:r + 1, :].partition_broadcast(128), reads=['cumd'], writes=[cbn])
                        P.I('dve', 'tensor_scalar', out=junk[:], in0=cb_[:], scalar1=slot[:, 0:1], scalar2=0.0, op0=ALU.is_le, op1=ALU.add,
                            accum_out=idxf[:, r, 0:1], reads=[cbn, 'slot'], writes=['junk', 'idxf'])
                        P.I('act', 'activation', out=junk2[:], in_=cb_[:], func=AF.Sign, scale=-1.0, bias=sloth[:, 1:2],
                            accum_out=idxf[:, r, 1:2], reads=[cbn, 'sloth'], writes=['junk2', 'idxf_a'])
                if not OPT_RT:
                    P.I('dve', 'tensor_scalar', out=idxf[:, :, 1:2], in0=idxf[:, :, 1:2], scalar1=0.5, scalar2=float(T) / 2, op0=ALU.mult, op1=ALU.add,
                        reads=['idxf', 'idxf_a'], writes=['idxf'])
                for b in range(NB):
                    if b > 0:
                        P.I('dve', 'tensor_scalar', out=idxf[:, b * NE:(b + 1) * NE, :], in0=idxf[:, b * NE:(b + 1) * NE, :], scalar1=float(b * T),
                            scalar2=None, op0=ALU.add, reads=['idxf'], writes=['idxf'])
                P.I('dve', 'tensor_copy', out=idx_i[:], in_=idxf[:], reads=['idxf'], writes=['idx_i'])
                dump("idxf", idxf[:], [128, R, 2], F32, ['idxf'])
                P.end_scope()

            with contextlib.ExitStack() as ef:
                xs = sb(ef, "xs", [128, NJ, D], BF16)
                xsT = sb(ef, "xsT", [128, 8, NSL], BF16)
                hidT = sb(ef, "hidT", [128, 8, NSL], BF16)
                gsel = sb(ef, "gsel", [128, NJ, NE], F32)
                sgm = [sb(ef, "sgm%d" % i, [128, 512], F32) for i in range(2)]
                yg = [sb(ef, "yg%d" % i, [128, D], F32) for i in range(3)]
                nn = min(512, NSL)
                nh = NSL // nn

                gsel2 = [gsel, sb(ef, "gselb", [128, NJ, NE], F32)]

                def wnames(nm):
                    return ['%s_k%d' % (nm, k2) for k2 in range(4)]

                def gathers(e):
                    gs = gsel2[e % 2]
                    for j in range(NJ):
                        b = j // 2
                        st_ = j % 2
                        r = b * NE + e
                        P.dma('pool', (lambda eng, o=xs[:, j, :], ix=idx_i[:, r, st_:st_ + 1]: eng.indirect_dma_start(
                            out=o, out_offset=None, in_=h2d, in_offset=bass.IndirectOffsetOnAxis(ap=ix, axis=0))),
                            reads=['idx_i', 'h2d'], writes=['xs%d' % j])
                        P.dma('pool', (lambda eng, o=gs[:, j, :], ix=idx_i[:, r, st_:st_ + 1]: eng.indirect_dma_start(
                            out=o, out_offset=None, in_=affd, in_offset=bass.IndirectOffsetOnAxis(ap=ix, axis=0))),
                            reads=['idx_i', 'affd'], writes=['gsel%d_%d' % (e % 2, j)])

                gathers(0)
                ygi = 0
                for e in range(NE):
                    i2 = e % 2
                    gsel = gsel2[e % 2]
                    if e >= 1 and e + 1 < NE:
                        load_w(e + 1)
                    wgn, wun, wdn = wnames('wg%d' % i2), wnames('wu%d' % i2), wnames('wd%d' % i2)
                    for j in range(NJ):
                        for half in range(2):
                            bi = ((2 * j + half) % 4) if OM1 else half
                            trn = 'BK%d' % bi
                            trb = BKb[bi][:, 0:512].rearrange("p (c t) -> p c t", t=128)
                            for q4 in range(4):
                                kc = half * 4 + q4
                                P.I('pe', 'transpose', trb[:, q4, :], xs[:, j, kc * 128:(kc + 1) * 128], ident_b[:],
                                    reads=['xs%d' % j, 'ident_b'], writes=[trn])
                            if OM1 and half == 1:
                                P.I('dve', 'tensor_copy', out=xsT[:, half * 4:(half + 1) * 4, j * 128:(j + 1) * 128], in_=trb,
                                    reads=[trn], writes=['xsT'])
                            else:
                                P.I('act', 'activation', out=xsT[:, half * 4:(half + 1) * 4, j * 128:(j + 1) * 128], in_=trb, func=AF.Copy,
                                    reads=[trn], writes=['xsT'])
                    if e + 1 < NE:
                        gathers(e + 1)
                    for fc in range(8):
                        for hs in range(nh):
                            k_ = (fc * nh + hs) % 2
                            pg, pgn = banks[0 + k_]
                            pu, pun = banks[2 + k_]
                            for kc in range(8):
                                P.I('pe', 'matmul', pg[:, 0:nn], wg[i2][:, kc, fc * 128:(fc + 1) * 128], xsT[:, kc, hs * nn:(hs + 1) * nn],
                                    start=(kc == 0), stop=(kc == 7), reads=wgn + ['xsT'], writes=[pgn])
                            for kc in range(8):
                                P.I('pe', 'matmul', pu[:, 0:nn], wu[i2][:, kc, fc * 128:(fc + 1) * 128], xsT[:, kc, hs * nn:(hs + 1) * nn],
                                    start=(kc == 0), stop=(kc == 7), reads=wun + ['xsT'], writes=[pun])
                            P.I('act', 'activation', out=sgm[k_][:, 0:nn], in_=pg[:, 0:nn], func=AF.Silu, reads=[pgn], writes=['sgm%d' % k_])
                            P.I('dve', 'tensor_tensor', out=hidT[:, fc, hs * nn:(hs + 1) * nn], in0=sgm[k_][:, 0:nn], in1=pu[:, 0:nn], op=ALU.mult,
                                reads=['sgm%d' % k_, pun], writes=['hidT'])
                    for j in range(NJ):
                        b = j // 2
                        st_ = j % 2
                        r = b * NE + e
                        y_, yn = yg[ygi % 3], 'yg%d' % (ygi % 3)
                        ygi += 1
                        for half in range(2):
                            py, pyn = banks[4 + half]
                            for kc in range(8):
                                P.I('pe', 'matmul', py, hidT[:, kc, j * 128:(j + 1) * 128], wd[i2][:, kc, half * 512:(half + 1) * 512],
                                    start=(kc == 0), stop=(kc == 7), reads=wdn + ['hidT'], writes=[pyn])
                            P.I('dve', 'scalar_tensor_tensor', out=y_[:, half * 512:(half + 1) * 512], in0=py, scalar=gsel[:, j, e:e + 1],
                                in1=g2bc[:, b, half * 512:(half + 1) * 512], op0=ALU.mult, op1=ALU.mult,
                                reads=[pyn, 'gsel%d_%d' % (e % 2, j), 'g2bc'], writes=[yn])
                        P.dma('pool', (lambda eng, i_=y_[:, :], ix=idx_i[:, r, st_:st_ + 1]: eng.indirect_dma_start(
                            out=outf, out_offset=bass.IndirectOffsetOnAxis(ap=ix, axis=0), in_=i_, in_offset=None, compute_op=ALU.add)),
                            reads=[yn, 'idx_i'], writes=['out_b%d' % b])
                P.barrier()
        P.barrier()
        P.emit()
    return nc


_CONSTS = None


def _prep_core(inp, b0, NB):
    global _CONSTS
    if _CONSTS is None:
        _CONSTS = host_consts()
    f = lambda a: np.ascontiguousarray(np.asarray(a, dtype=np.float32))
    cstack = np.concatenate([inp["c"][b0:b0 + NB], inp["c_ctx"][None, :]], axis=0)
    cT = f(cstack.reshape(NB + 1, 8, 128).transpose(2, 1, 0))
    bdec = np.stack([inp["b_decay_fwd"][0].reshape(2, 128).T, inp["b_decay_bwd"][0].reshape(2, 128).T], axis=1)
    m = dict(
        x=f(inp["x"][b0:b0 + NB]), ctx=f(inp["ctx"][b0:b0 + NB]), cT=cT,
        w_mod=f(inp["w_mod"][0]), b_modT=f(inp["b_mod"][0].reshape(48, 128).T), n1gT=f(inp["norm1_g"][0].reshape(8, 128).T),
        w_in=f(inp["w_in"][0]), qng=f(inp["q_norm_g"][0].reshape(64, 1)), kng=f(inp["k_norm_g"][0].reshape(64, 1)),
        sink=f(inp["attn_sink"][0]), wdf=f(inp["w_decay_fwd"][0]), wdb=f(inp["w_decay_bwd"][0]), bdec=f(bdec),
        glag=f(inp["gla_norm_g"][0]), w_out=f(inp["w_out"][0]), n2g=f(inp["norm2_g"][0]), w_router=f(inp["w_router"][0]),
        weg=f(inp["w_e_gate"][0]), weu=f(inp["w_e_up"][0]), wed=f(inp["w_e_down"][0]),
    )
    m.update(_CONSTS)
    return m


def kernel(**inputs):
    inp = {k: np.asarray(v) for k, v in inputs.items()}
    B = inp["x"].shape[0]
    NB = B // NCORES
    nc = build(NB)
    in_maps = [_prep_core(inp, i * NB, NB) for i in range(NCORES)]
    res = run_bass_kernel_spmd(nc, in_maps, core_ids=list(range(NCORES)))
    outs = [np.asarray(r["out"]).reshape(NB, T, D) for r in res.results]
    return np.concatenate(outs, axis=0).astype(np.float32)
```

```python
import contextlib
import os
import numpy as np
BIS = int(os.environ.get('BIS', '99'))
OPT_REC = int(os.environ.get('OPT_REC', '0'))
OPT_CPREP = int(os.environ.get('OPT_CPREP', '1'))
OPT_REORD = int(os.environ.get('OPT_REORD', '1'))
OPT_RT = int(os.environ.get('OPT_RT', '0'))
OPT_I16 = int(os.environ.get('OPT_I16', '1'))
OA1 = int(os.environ.get('OA1', '0'))
OA4 = int(os.environ.get('OA4', '1'))
OB1 = int(os.environ.get('OB1', '0'))
OC7 = int(os.environ.get('OC7', '1'))
OC8 = int(os.environ.get('OC8', '1'))
OAP = int(os.environ.get('OAP', '0'))
OG2 = int(os.environ.get('OG2', '0'))
OP0 = int(os.environ.get('OP0', '0'))
OM1 = int(os.environ.get('OM1', '1'))
OCL = int(os.environ.get('OCL', '1'))
OFW = int(os.environ.get('OFW', '0'))
OPAIR = int(os.environ.get('OPAIR', '1'))
OC9 = int(os.environ.get('OC9', '0'))
OATT = int(os.environ.get('OATT', '3'))
OH7 = int(os.environ.get('OH7', '0'))
NOSELF = os.environ.get('NOSELF', '')
OB2 = int(os.environ.get('OB2', '0'))
OB3 = int(os.environ.get('OB3', '0'))
OB4 = int(os.environ.get('OB4', '0'))
OB5 = int(os.environ.get('OB5', '0'))
OA5 = int(os.environ.get('OA5', '0'))
OA6 = int(os.environ.get('OA6', '0'))
import concourse.bass as bass
import concourse.mybir as mybir
from concourse.alu_op_type import AluOpType as ALU
from concourse.bass_utils import run_bass_kernel_spmd

AF = mybir.ActivationFunctionType
AX = mybir.AxisListType
F32 = mybir.dt.float32
BF16 = mybir.dt.bfloat16
I32 = mybir.dt.int32
I16 = mybir.dt.int16

NCORES = 8
D = 1024
T = 2048
C = 256
S = T + C
NE = 16
CAP = 256
EPS = 1e-6

CENG = ('pe', 'act', 'dve', 'pool')
ALLENG = ('pe', 'act', 'dve', 'pool', 'sp')


class Prog:
    def __init__(self, nc):
        self.nc = nc
        self.ops = {e: [] for e in ALLENG}
        self.cnt = {e: 0 for e in CENG}
        self.seen = {e: {} for e in ALLENG}
        self.last_w = {}
        self.readers = {}
        lanes_per_queue = {'sp': 14, 'pool': 14, 'act': 2, 'poolw': 12}
        self.lanes = {q: ['L_%s_%d' % (q, i) for i in range(n)] for q, n in lanes_per_queue.items()}
        self.lane_cnt = {l: 0 for q in self.lanes for l in self.lanes[q]}
        self.lane_rr = {q: 0 for q in self.lanes}
        self.semnames = ['E_' + e for e in CENG] + [l for q in self.lanes for l in self.lanes[q]]
        self.sems = None
        self.flushed = {e: 0 for e in ALLENG}
        self.scoped_bases = set()
        self.cls = {}
        self.live_scoped = set()
        self.fence = {}

    def _touch(self, names):
        for n in names:
            c = self.cls.get(n)
            if c is None:
                c = any(n.startswith(b_) for b_ in self.scoped_bases)
                self.cls[n] = c
            if c and n not in self.live_scoped:
                self.live_scoped.add(n)
                self.last_w.pop(n, None)
                self.readers[n] = dict(self.fence)

    def end_scope(self):
        for e in CENG:
            if self.cnt[e] > 0:
                self.fence['E_' + e] = self.cnt[e]
        for l, c in self.lane_cnt.items():
            if c > 0:
                self.fence[l] = 16 * c
        for n in self.live_scoped:
            self.last_w.pop(n, None)
            self.readers.pop(n, None)
        self.live_scoped = set()

    def _deps(self, eng, reads, writes):
        self._touch(reads)
        self._touch(writes)
        need = {}

        def add(tok):
            if tok is None:
                return
            s, v = tok
            if eng == 'pe' and s == 'E_pe':
                return
            if NOSELF and eng in NOSELF.split(',') and s == 'E_' + eng:
                return
            if need.get(s, 0) < v:
                need[s] = v
        for r in reads:
            add(self.last_w.get(r))
        for w in writes:
            add(self.last_w.get(w))
            for t in self.readers.get(w, {}).items():
                add(t)
        return need

    def _commit(self, tok, reads, writes):
        for w in writes:
            self.last_w[w] = tok
            self.readers[w] = {}
        for r in reads:
            if r in writes:
                continue
            d = self.readers.setdefault(r, {})
            if d.get(tok[0], 0) < tok[1]:
                d[tok[0]] = tok[1]

    def _filter(self, eng, need):
        out = []
        seen = self.seen[eng]
        for s, v in need.items():
            if seen.get(s, 0) < v:
                seen[s] = v
                out.append((s, v))
        return out

    def op(self, eng, fn, reads=(), writes=()):
        need = self._deps(eng, reads, writes)
        waits = self._filter(eng, need)
        self.cnt[eng] += 1
        tok = ('E_' + eng, self.cnt[eng])
        self.ops[eng].append((waits, fn, tok[0], 1))
        self._commit(tok, reads, writes)
        return tok

    def I(self, eng, method, *args, reads=(), writes=(), **kw):
        return self.op(eng, lambda e: getattr(e, method)(*args, **kw), reads, writes)

    def dma(self, q0, fn, reads=(), writes=()):
        q = q0
        q0 = 'pool' if q == 'poolw' else q
        need = self._deps(q0, reads, writes)
        lane = self.lanes[q][self.lane_rr[q] % len(self.lanes[q])]
        self.lane_rr[q] += 1
        prior = 16 * self.lane_cnt[lane]
        if prior > 0 and need.get(lane, 0) < prior:
            need[lane] = prior
        waits = self._filter(q0, need)
        self.lane_cnt[lane] += 1
        tok = (lane, prior + 16)
        self.ops[q0].append((waits, fn, lane, 16))
        self._commit(tok, reads, writes)
        return tok

    def DMA(self, q, out, in_, reads=(), writes=(), **kw):
        return self.dma(q, lambda e: e.dma_start(out=out, in_=in_, **kw), reads, writes)

    def wait_all(self, eng):
        need = {}
        for e in CENG:
            if e != eng and self.cnt[e] > 0:
                need['E_' + e] = self.cnt[e]
        for l, c in self.lane_cnt.items():
            if c > 0:
                need[l] = 16 * c
        waits = self._filter(eng, need)
        if waits:
            self.ops[eng].append((waits, None, None, 0))

    def barrier(self):
        for e in ALLENG:
            self.wait_all(e)

    def alloc_sems(self, es):
        self.sems = {n: es.enter_context(self.nc.semaphore(n)) for n in self.semnames}

    def emit(self):
        nc = self.nc
        prog = self

        def replay(ename, eng):
            lst = prog.ops[ename]
            for waits, fn, sname, inc in lst[prog.flushed[ename]:]:
                for s, v in waits:
                    eng.wait_ge(prog.sems[s], v)
                if fn is not None:
                    ins = fn(eng)
                    ins.then_inc(prog.sems[sname], inc)
            prog.flushed[ename] = len(lst)

        with nc.Block() as block:
            @block.tensor
            def _(eng):
                replay('pe', eng)

            @block.scalar
            def _(eng):
                replay('act', eng)

            @block.vector
            def _(eng):
                replay('dve', eng)

            @block.gpsimd
            def _(eng):
                replay('pool', eng)

            @block.sync
            def _(eng):
                replay('sp', eng)


def host_consts():
    p = np.arange(128)
    d = p % 64
    axis = d // 32
    half = (d % 32) // 16
    freq = d % 16
    inv = (np.float32(10000.0) ** (-(freq.astype(np.float32)) / np.float32(16))).astype(np.float32)
    t = np.arange(T)
    row = (t // 64).astype(np.float32)
    col = (t % 64).astype(np.float32)
    pos = np.where(axis[:, None] == 0, row[None, :], col[None, :]).astype(np.float32)
    ang = (pos * inv[:, None]).astype(np.float32)
    cos = np.ones((128, S), np.float32)
    sin = np.zeros((128, S), np.float32)
    cos[:, C:] = np.cos(ang)
    sgn = np.where(half == 0, -1.0, 1.0).astype(np.float32)
    sin[:, C:] = np.sin(ang) * sgn[:, None]
    partner = (p // 64) * 64 + np.where(half == 0, d + 16, d - 16)
    perm = np.zeros((128, 128), np.float32)
    perm[partner, p] = 1.0
    bones = (p[:, None] // 64 == p[None, :] // 64).astype(np.float32)
    ident = np.eye(128, dtype=np.float32)
    i = p[:, None]
    q = p[None, :]
    mattn = np.stack([(i >= q), (i <= q)], axis=1).astype(np.float32)
    same = (i // 64 == q // 64)
    mgla = np.stack([same & (i <= q), same & (i >= q)], axis=1).astype(np.float32)
    rmask = np.ones((128, 512), np.float32)
    rmask[:, ::64] = 0.0
    slotid = np.stack([p, p + 128], axis=1).astype(np.float32)
    return dict(c_cos=cos, c_sin=sin, c_perm=perm, c_bones=bones, c_ident=ident,
                c_mattn=np.ascontiguousarray(mattn), c_mgla=np.ascontiguousarray(mgla),
                c_rmask=rmask, c_slotid=slotid)


def build(NB, stage=99, dumps=()):
    nc = bass.Bass("TRN2", target_bir_lowering=False)
    R = NB * NE
    NJ = NB * 2
    NSL = NB * CAP

    def din(name, shape, dt=F32):
        return nc.dram_tensor(name, list(shape), dt, kind="ExternalInput").ap()

    x = din("x", [NB, T, D])
    ctx = din("ctx", [NB, C, D])
    cT = din("cT", [128, 8, NB + 1])
    w_mod = din("w_mod", [D, 6 * D])
    b_modT = din("b_modT", [128, 48])
    n1gT = din("n1gT", [128, 8])
    w_in = din("w_in", [D, 2336])
    qng = din("qng", [64, 1])
    kng = din("kng", [64, 1])
    sink = din("sink", [8])
    wdf = din("wdf", [16, 256])
    wdb = din("wdb", [16, 256])
    bdec = din("bdec", [128, 2, 2])
    glag = din("glag", [128])
    w_out = din("w_out", [D, D])
    n2g = din("n2g", [D])
    w_router = din("w_router", [D, NE])
    weg = din("weg", [NE, D, D])
    weu = din("weu", [NE, D, D])
    wed = din("wed", [NE, D, D])
    c_cos = din("c_cos", [128, S])
    c_sin = din("c_sin", [128, S])
    c_perm = din("c_perm", [128, 128])
    c_bones = din("c_bones", [128, 128])
    c_ident = din("c_ident", [128, 128])
    c_mattn = din("c_mattn", [128, 2, 128])
    c_mgla = din("c_mgla", [128, 2, 128])
    c_rmask = din("c_rmask", [128, 512])
    c_slotid = din("c_slotid", [128, 2])
    out = nc.dram_tensor("out", [NB, T, D], F32, kind="ExternalOutput").ap()
    outf = out.rearrange("b t d -> (b t) d")
    h2d = nc.dram_tensor("h2d", [NB * T, D], BF16, kind="Internal").ap()
    affd = nc.dram_tensor("affd", [NB * T, NE], F32, kind="Internal").ap()
    cumd = nc.dram_tensor("cumd", [R, T], I16 if OPT_I16 else F32, kind="Internal").ap()

    P = Prog(nc)
    dump_aps = {}

    def dump(name, src_ap, shape, dt, reads):
        if name not in dumps:
            return
        dd = nc.dram_tensor("dbg_" + name, list(shape), dt, kind="ExternalOutput").ap()
        P.DMA('sp', dd, src_ap, reads=reads, writes=['dbg_' + name])

    with contextlib.ExitStack() as es:
        P.alloc_sems(es)

        uniq = [0]
        persist_es = [None]

        def sb(es_, name, shape, dt):
            uniq[0] += 1
            if es_ is not es and es_ is not persist_es[0]:
                P.scoped_bases.add(name)
            return es_.enter_context(nc.sbuf_tensor("%s_%d" % (name, uniq[0]), list(shape), dt))

        PP = [es.enter_context(nc.psum_tensor("PP%d" % i, [128, 1024], F32)) for i in range(2)]
        BK = [es.enter_context(nc.psum_tensor("BK%d" % i, [128, 512], F32)) for i in range(4)]
        banks = []
        for i in range(2):
            banks.append((PP[i][:, 0:512], 'PP%da' % i))
            banks.append((PP[i][:, 512:1024], 'PP%db' % i))
        for i in range(4):
            banks.append((BK[i][:, :], 'BK%d' % i))
        PPb = [PP[i].bitcast(BF16) for i in range(2)]
        BKb = [BK[i].bitcast(BF16) for i in range(4)]

        ident_b = sb(es, "ident_b", [128, 128], BF16)
        ident_f = sb(es, "ident_f", [128, 128], F32)
        modT = sb(es, "modT", [128, 48, NB + 1], F32)
        aff = sb(es, "aff", [128, NB, 16, NE], F32)
        affT = sb(es, "affT", [R, T], F32)
        P.DMA('pool', ident_b[:], c_ident, writes=['ident_b'])
        P.DMA('sp', ident_f[:], c_ident, writes=['ident_f'])

        with contextlib.ExitStack() as e0:
            cs_f = sb(e0, "cs_f", [128, 8, NB + 1], F32)
            cs = sb(e0, "cs", [128, 8, NB + 1], BF16)
            bmT = sb(e0, "bmT", [128, 48], F32)
            wm = [sb(e0, "wm%d" % i, [128, 8, 1024], BF16) for i in range(2)]
            P.DMA('sp', cs_f[:], cT, writes=['cs_f'])
            P.DMA('sp', bmT[:], b_modT, writes=['bmT'])
            P.I('act', 'activation', out=cs[:], in_=cs_f[:], func=AF.Silu, reads=['cs_f'], writes=['cs'])
            if OP0:
                wmf = [sb(e0, "wmf%d" % i, [128, 4, 1024], F32) for i in range(2)]
            for j in range(6):
                hw = OP0 and (j % 2 == 1)
                w_, wn = wm[j % 2], 'wm%d' % (j % 2)
                for k2 in range(2):
                    src = w_mod[k2 * 512:(k2 + 1) * 512, j * 1024:(j + 1) * 1024].rearrange("(k p) c -> p k c", p=128)
                    if hw:
                        P.DMA('sp', wmf[k2][:], src, writes=['wmf%d' % k2])
                        if k2 == 0:
                            P.I('act', 'activation', out=w_[:, 0:4, :], in_=wmf[0][:], func=AF.Copy, reads=['wmf0'], writes=[wn + '_k0'])
                        else:
                            P.I('dve', 'tensor_copy', out=w_[:, 4:8, :], in_=wmf[1][:], reads=['wmf1'], writes=[wn + '_k1'])
                    else:
                        P.DMA('pool', w_[:, 4 * k2:4 * k2 + 4, :], src, writes=[wn + '_k%d' % k2])
                pm, pmn = banks[4 + (j % 2)]
                pmv = pm[:, 0:64].rearrange("p (f c) -> p f c", c=8)
                for fc in range(8):
                    for kc in range(8):
                        P.I('pe', 'matmul', pmv[:, fc, 0:NB + 1], w_[:, kc, fc * 128:(fc + 1) * 128], cs[:, kc, :],
                            start=(kc == 0), stop=(kc == 7), reads=[wn + '_k0', wn + '_k1', 'cs'], writes=[pmn])
                P.I('dve', 'tensor_tensor', out=modT[:, j * 8:(j + 1) * 8, :], in0=pmv[:, :, 0:NB + 1],
                    in1=bmT[:, j * 8:(j + 1) * 8].unsqueeze(2).to_broadcast([128, 8, NB + 1]), op=ALU.add,
                    reads=[pmn, 'bmT'], writes=['modT'])
            P.barrier()
        dump("modT", modT[:], [128, 48, NB + 1], F32, ['modT'])

        with contextlib.ExitStack() as em:
            persist_es[0] = em
            perm_b = sb(em, "perm_b", [128, 128], BF16)
            bones_b = sb(em, "bones_b", [128, 128], BF16)
            mattn = sb(em, "mattn", [128, 2, 128], BF16)
            mgla = sb(em, "mgla", [128, 2, 128], BF16)
            rmask = sb(em, "rmask", [128, 512], BF16)
            n1g = sb(em, "n1g", [128, 8], F32)
            scale1 = sb(em, "scale1", [128, 8, NB + 1], F32)
            gqc = sb(em, "gqc", [128, 1], F32)
            gkc = sb(em, "gkc", [128, 1], F32)
            es_t = sb(em, "es_t", [128, 8, 1], F32)
            nbd = sb(em, "nbd", [128, 2, 2], F32)
            glab = sb(em, "glab", [128, 128], F32)
            wr = sb(em, "wr", [128, 8, NE], F32)
            wdx = sb(em, "wdx", [32, 2, 256], BF16)
            hT = sb(em, "hT", [128, 8, S], BF16)
            mixT = sb(em, "mixT", [128, 8, T], BF16)
            wsl = sb(em, "wsl", [128, 8, 1024], BF16)
            rep = [sb(em, "rep%d" % i, [128, 128], F32) for i in range(2)]

            for dst, src, nm in ((perm_b, c_perm, 'perm_b'), (bones_b, c_bones, 'bones_b'),
                                 (mattn, c_mattn, 'mattn'), (mgla, c_mgla, 'mgla'), (rmask, c_rmask, 'rmask')):
                P.DMA('pool', dst[:], src, writes=[nm])
            P.DMA('sp', n1g[:], n1gT, writes=['n1g'])
            P.DMA('sp', gqc[0:64, :], qng, writes=['gqc'])
            P.DMA('sp', gqc[64:128, :], qng, writes=['gqc'])
            P.DMA('sp', gkc[0:64, :], kng, writes=['gkc'])
            P.DMA('sp', gkc[64:128, :], kng, writes=['gkc'])
            P.DMA('sp', es_t[:].rearrange("p h o -> p (h o)"), sink.partition_broadcast(128), writes=['es_t'])
            P.DMA('sp', nbd[:], bdec, writes=['nbd'])
            P.DMA('sp', glab[:], glag.partition_broadcast(128), writes=['glab'])
            P.DMA('sp', wr[:], w_router.rearrange("(k p) e -> p k e", p=128), writes=['wr'])
            P.I('dve', 'memset', wdx[:], 0.0, writes=['wdx'])
            P.DMA('pool', wdx[0:16, 0, :], wdf, writes=['wdx'])
            P.DMA('pool', wdx[16:32, 1, :], wdb, writes=['wdx'])
            P.I('dve', 'tensor_scalar', out=gqc[:], in0=gqc[:], scalar1=0.125, scalar2=None, op0=ALU.mult, reads=['gqc'], writes=['gqc'])
            P.I('act', 'activation', out=es_t[:], in_=es_t[:], func=AF.Exp, reads=['es_t'], writes=['es_t'])
            P.I('dve', 'tensor_scalar', out=nbd[:], in0=nbd[:], scalar1=-1.0, scalar2=None, op0=ALU.mult, reads=['nbd'], writes=['nbd'])
            P.I('dve', 'tensor_scalar', out=scale1[:], in0=modT[:, 8:16, :], scalar1=1.0, scalar2=None, op0=ALU.add,
                reads=['modT'], writes=['scale1'])
            P.I('dve', 'tensor_tensor', out=scale1[:], in0=scale1[:], in1=n1g[:].unsqueeze(2).to_broadcast([128, 8, NB + 1]),
                op=ALU.mult, reads=['scale1', 'n1g'], writes=['scale1'])

            repi = [0]

            def bcast_row(dst, dstname, chunk0, col):
                for half in range(2):
                    bk, bkn = banks[6 + half]
                    for q4 in range(4):
                        kc = half * 4 + q4
                        r_ = rep[repi[0] % 2]
                        rn = 'rep%d' % (repi[0] % 2)
                        repi[0] += 1
                        P.I('dve', 'tensor_copy', out=r_[:], in_=modT[:, chunk0 + kc, col:col + 1].to_broadcast([128, 128]),
                            reads=['modT'], writes=[rn])
                        P.I('pe', 'matmul', bk[:, q4 * 128:(q4 + 1) * 128], r_[:], ident_f[:], start=True, stop=True,
                            reads=[rn, 'ident_f'], writes=[bkn])
                    P.I('act', 'activation', out=dst[:, half * 512:(half + 1) * 512], in_=bk, func=AF.Copy,
                        reads=[bkn], writes=[dstname])

            g1bc = sb(em, "g1bc", [128, D], F32)
            wst = [sb(em, "wst%d" % i, [128, D], F32) for i in range(2)]

            def c_prep(b):
                wO = wsl
                bcast_row(g1bc, 'g1bc', 16, b)
                for kc in range(8):
                    P.DMA('sp', wst[kc % 2][:], w_out[kc * 128:(kc + 1) * 128, :], writes=['wst%d' % (kc % 2)])
                    P.I('dve' if kc % 2 == 0 else 'pool', 'tensor_tensor', out=wO[:, kc, :], in0=wst[kc % 2][:], in1=g1bc[:], op=ALU.mult,
                        reads=['wst%d' % (kc % 2), 'g1bc'], writes=['wsl'])

            def h_partA(b, ti):
                xt_ = xt[ti % 2]
                xtn = 'xt%d' % (ti % 2)
                xn_ = xn[ti % 2]
                xnn = 'xn%d' % (ti % 2)
                st_ = st4[ti % 2]
                stn = 'st4_%d' % (ti % 2)
                src = ctx[b, ti * 128:(ti + 1) * 128, :] if ti < 2 else x[b, (ti - 2) * 128:(ti - 1) * 128, :]
                P.DMA('sp', xt_[:], src, writes=[xtn])
                P.I('act', 'activation', out=xn_[:], in_=xt_[:], func=AF.Square, accum_out=st_[:, 0:1],
                    reads=[xtn], writes=[xnn, stn])
                P.I('act', 'activation', out=st_[:, 1:2], in_=st_[:, 0:1], func=AF.Ln, scale=1.0 / D, bias=EPS,
                    reads=[stn], writes=[stn])
                P.I('act', 'activation', out=st_[:, 2:3], in_=st_[:, 1:2], func=AF.Exp, scale=-0.5,
                    reads=[stn], writes=[stn])
                if OH7:
                    return
                h_partA2(b, ti)

            def h_partA2(b, ti):
                xt_ = xt[ti % 2]
                xtn = 'xt%d' % (ti % 2)
                xn_ = xn[ti % 2]
                xnn = 'xn%d' % (ti % 2)
                st_ = st4[ti % 2]
                stn = 'st4_%d' % (ti % 2)
                P.I('dve', 'tensor_scalar', out=xn_[:], in0=xt_[:], scalar1=st_[:, 2:3], scalar2=None, op0=ALU.mult,
                    reads=[xtn, stn], writes=[xnn])
                tpn = 'BK%d' % (2 + ti % 2)
                tp = BKb[2 + ti % 2][:, 0:1024].rearrange("p (k t) -> p k t", t=128)
                for kc in range(8):
                    P.I('pe', 'transpose', tp[:, kc, :], xn_[:, kc * 128:(kc + 1) * 128], ident_b[:],
                        reads=[xnn, 'ident_b'], writes=[tpn])

            def h_partB(b, ti):
                col = NB if ti < 2 else b
                tpn = 'BK%d' % (2 + ti % 2)
                tp = BKb[2 + ti % 2][:, 0:1024].rearrange("p (k t) -> p k t", t=128)
                for kc in range(8):
                    if kc < 3:
                        P.I('act', 'activation', out=hT[:, kc, ti * 128:(ti + 1) * 128], in_=tp[:, kc, :], func=AF.Identity,
                            scale=scale1[:, kc, col:col + 1], bias=modT[:, kc, col:col + 1],
                            reads=[tpn, 'scale1', 'modT'], writes=['hT%d' % ti])
                    else:
                        P.I('dve', 'tensor_scalar', out=hT[:, kc, ti * 128:(ti + 1) * 128], in0=tp[:, kc, :],
                            scalar1=scale1[:, kc, col:col + 1], scalar2=modT[:, kc, col:col + 1], op0=ALU.mult, op1=ALU.add,
                            reads=[tpn, 'scale1', 'modT'], writes=['hT%d' % ti])

            for b in range(NB):
                with contextlib.ExitStack() as eh:
                    xt = [sb(eh, "xt%d" % i, [128, D], F32) for i in range(2)]
                    xn = [sb(eh, "xn%d" % i, [128, D], BF16) for i in range(2)]
                    st4 = [sb(eh, "st4_%d" % i, [128, 4], F32) for i in range(2)]
                    for ti in range(19):
                        if ti < 18:
                            h_partA(b, ti)
                        if ti >= 1:
                            h_partB(b, ti - 1)
                        if ti < 18 and OH7:
                            h_partA2(b, ti)
                    P.end_scope()
                if b == 0:
                    dump("hT", hT[:], [128, 8, S], BF16, ['hT%d' % i for i in range(18)])
                if stage <= 1:
                    continue
                hTall = ['hT%d' % i for i in range(18)]
                chunks = [(0, 256)] + [(C + 512 * i, 512) for i in range(4)]

                with contextlib.ExitStack() as ea:
                    qT = sb(ea, "qT", [128, 4, T], BF16)
                    kT = sb(ea, "kT", [128, 2, S], BF16)
                    vaug = sb(ea, "vaug", [128, 18, 2, 66], BF16)
                    u_ = [sb(ea, "u%d" % i, [128, 512], BF16) for i in range(3 if OAP else 2)]
                    sq_ = [sb(ea, "sq%d" % i, [128, 512], BF16) for i in range(3 if OAP else 2)]
                    lnt = sb(ea, "lnt", [128, 512], F32)
                    rr_ = [sb(ea, "rr%d" % i, [128, 512], F32) for i in range(3 if OAP else 2)]
                    t1_ = [sb(ea, "t1_%d" % i, [128, 512], F32) for i in range(3 if OAP else 2)]
                    t2_ = [sb(ea, "t2_%d" % i, [128, 512], F32) for i in range(3 if OAP else 2)]
                    pt_ = [sb(ea, "pt%d" % i, [128, 640], BF16) for i in range((max(OATT, 2) + 1) if OATT else 2)]
                    den = sb(ea, "den", [128, 8, 1], F32)
                    rec = sb(ea, "rec", [128, 8, 1], F32)
                    ao = sb(ea, "ao", [128, 8, 64], BF16)
                    cos_b = sb(ea, "cos_b", [128, S], BF16)
                    sin_b = sb(ea, "sin_b", [128, S], BF16)
                    for dst, src, nm in ((cos_b, c_cos, 'cos_b'), (sin_b, c_sin, 'sin_b')):
                        P.DMA('pool', dst[:], src, writes=[nm])
                    wA = wsl
                    P.DMA('pool', wA[:, :, 0:512], w_in[:, 0:512].rearrange("(k p) c -> p k c", p=128), writes=['wsl'])
                    for hh in range(2):
                        for dup in range(2):
                            o = 512 + hh * 128 + dup * 64
                            P.DMA('pool', wA[:, :, o:o + 64], w_in[:, 512 + hh * 64:512 + (hh + 1) * 64].rearrange("(k p) c -> p k c", p=128),
                                  writes=['wsl'])
                    P.DMA('pool', wA[:, :, 768:896], w_in[:, 640:768].rearrange("(k p) c -> p k c", p=128), writes=['wsl'])
                    P.I('pool', 'memset', vaug[:, :, :, 64:65], 1.0, writes=['vaug'])
                    def a_p1(s0, n, mc, i2, ib=None):
                        tiles = list(range(s0 // 128, (s0 + n) // 128))
                        hTr = ['hT%d' % i for i in tiles]
                        pj, pjn = banks[i2]
                        i2 = i2 if ib is None else ib
                        for kc in range(8):
                            P.I('pe', 'matmul', pj[:, 0:n], wA[:, kc, mc * 128:(mc + 1) * 128], hT[:, kc, s0:s0 + n],
                                start=(kc == 0), stop=(kc == 7), reads=['wsl'] + hTr, writes=[pjn])
                        gcol = gqc if mc < 4 else gkc
                        u, un = u_[i2], 'u%d' % i2
                        sq, sqn = sq_[i2], 'sq%d' % i2
                        P.I('act', 'activation', out=u[:, 0:n], in_=pj[:, 0:n], func=AF.Copy, scale=gcol[:, 0:1],
                            reads=[pjn, 'gqc', 'gkc'], writes=[un])
                        P.I('act', 'activation', out=sq[:, 0:n], in_=pj[:, 0:n], func=AF.Square, reads=[pjn], writes=[sqn])

                    def a_p2(s0, n, mc, i2, ib=None):
                        i2 = i2 if ib is None else ib
                        pss, pssn = banks[2]
                        ppu, ppun = banks[3]
                        isq = mc < 4
                        dest = qT[:, mc, s0 - C:s0 - C + n] if isq else kT[:, mc - 4, s0:s0 + n]
                        destn = 'qT' if isq else 'kT'
                        u, un = u_[i2], 'u%d' % i2
                        sq, sqn = sq_[i2], 'sq%d' % i2
                        rr, rrn = rr_[i2], 'rr%d' % i2
                        t1, t1n = t1_[i2], 't1_%d' % i2
                        t2, t2n = t2_[i2], 't2_%d' % i2
                        P.I('pe', 'matmul', pss[:, 0:n], bones_b[:], sq[:, 0:n], start=True, stop=True,
                            reads=['bones_b', sqn], writes=[pssn])
                        P.I('pe', 'matmul', ppu[:, 0:n], perm_b[:], u[:, 0:n], start=True, stop=True,
                            reads=['perm_b', un], writes=[ppun])
                        P.I('act', 'activation', out=lnt[:, 0:n], in_=pss[:, 0:n], func=AF.Ln, scale=1.0 / 64, bias=EPS,
                            reads=[pssn], writes=['lnt'])
                        P.I('act', 'activation', out=rr[:, 0:n], in_=lnt[:, 0:n], func=AF.Exp, scale=-0.5,
                            reads=['lnt'], writes=[rrn])
                        P.I('dve' if OA1 else 'pool', 'tensor_tensor', out=t1[:, 0:n], in0=u[:, 0:n], in1=cos_b[:, s0:s0 + n], op=ALU.mult,
                            reads=[un, 'cos_b'], writes=[t1n])
                        P.I('dve', 'tensor_tensor', out=t2[:, 0:n], in0=ppu[:, 0:n], in1=sin_b[:, s0:s0 + n], op=ALU.mult,
                            reads=[ppun, 'sin_b'], writes=[t2n])
                        P.I('dve', 'tensor_tensor', out=t2[:, 0:n], in0=t2[:, 0:n], in1=t1[:, 0:n], op=ALU.add,
                            reads=[t2n, t1n], writes=[t2n])
                        P.I('dve', 'tensor_tensor', out=dest, in0=t2[:, 0:n], in1=rr[:, 0:n], op=ALU.mult,
                            reads=[t2n, rrn], writes=[destn])

                    def a_vproj(s0, n):
                        for ti in range(s0 // 128, (s0 + n) // 128):
                            bv, bvn = banks[4 + (ti % 2)]
                            for kc in range(8):
                                P.I('pe', 'matmul', bv[:, 0:128], hT[:, kc, ti * 128:(ti + 1) * 128], wA[:, kc, 768:896],
                                    start=(kc == 0), stop=(kc == 7), reads=['wsl', 'hT%d' % ti], writes=[bvn])
                            P.I('act', 'activation', out=vaug[:, ti, :, 0:64], in_=bv[:, 0:128].rearrange("p (h d) -> p h d", d=64),
                                func=AF.Copy, reads=[bvn], writes=['vaug'])

                    aitems = []
                    for (s0, n) in chunks:
                        mcs = ([0, 1, 2, 3] if s0 >= C else []) + [4, 5]
                        for mi_, mc in enumerate(mcs):
                            aitems.append((s0, n, mc, len(aitems) % 2, mi_ == len(mcs) - 1))
                    ALAG = 2 if OAP else 1
                    for i in range(len(aitems) + ALAG):
                        if i < len(aitems):
                            s0, n, mc, i2, lastmc = aitems[i]
                            a_p1(s0, n, mc, i2, ib=(i % 3 if OAP else None))
                            if lastmc:
                                a_vproj(s0, n)
                        if i >= ALAG:
                            s0, n, mc, i2, lastmc = aitems[i - ALAG]
                            a_p2(s0, n, mc, i2, ib=((i - ALAG) % 3 if OAP else None))
                    if b == 0:
                        dump("qT", qT[:], [128, 4, T], BF16, ['qT'])
                        dump("kT", kT[:], [128, 2, S], BF16, ['kT'])
                        dump("vaug", vaug[:], [128, 18, 2, 66], BF16, ['vaug'])
                    ao_ = [sb(ea, "aox%d" % i, [128, 8, 64], BF16) for i in range(2)]
                    pvA, pvAn = banks[4]
                    pvB, pvBn = banks[5]

                    def kbs_of(n_):
                        kbs = [(0, None), (1, None)]
                        if n_ > 0:
                            kbs.append((n_ + 1, 0))
                        kbs.append((n_ + 2, None))
                        if n_ < 15:
                            kbs.append((n_ + 3, 1))
                        return kbs

                    def emit_scores(n_, hq, pti=None):
                        kbs = kbs_of(n_)
                        nk = len(kbs)
                        kvh = hq // 4
                        ch = hq // 2
                        par = hq % 2
                        rows = slice(par * 64, par * 64 + 64)
                        sc = PP[hq % 2]
                        scn = ['PP%da' % (hq % 2), 'PP%db' % (hq % 2)]
                        pti = (hq % 2) if pti is None else pti
                        pt = pt_[pti]
                        ptn = 'pt%d' % pti
                        for j, (kt, mi) in enumerate(kbs):
                            P.I('pe', 'matmul', sc[:, j * 128:(j + 1) * 128], kT[rows, kvh, kt * 128:(kt + 1) * 128],
                                qT[rows, ch, n_ * 128:(n_ + 1) * 128], start=True, stop=True,
                                reads=['kT', 'qT'], writes=scn)
                        P.I('act', 'activation', out=pt[:, 0:nk * 128], in_=sc[:, 0:nk * 128], func=AF.Exp,
                            reads=scn, writes=[ptn])
                        for j, (kt, mi) in enumerate(kbs):
                            if mi is not None:
                                P.I('pool' if (OB5 and mi == 1) else 'dve', 'tensor_tensor', out=pt[:, j * 128:(j + 1) * 128], in0=pt[:, j * 128:(j + 1) * 128],
                                    in1=mattn[:, mi, :], op=ALU.mult, reads=[ptn, 'mattn'], writes=[ptn])

                    def emit_pv(n_, hq, pti=None, ptx=None):
                        kbs = kbs_of(n_)
                        nk = len(kbs)
                        kvh = hq // 4
                        pti = (hq % 2) if pti is None else pti
                        pt = pt_[pti]
                        ptn = 'pt%d' % pti
                        if ptx is not None:
                            pt, ptn = ptx
                        pv = (pvA if hq < 4 else pvB)[:, 0:264].rearrange("p (h d) -> p h d", d=66)
                        pvn = pvAn if hq < 4 else pvBn
                        for j, (kt, mi) in enumerate(kbs):
                            P.I('pe', 'matmul', pv[:, hq % 4, 0:65], pt[:, j * 128:(j + 1) * 128], vaug[:, kt, kvh, 0:65],
                                start=(j == 0), stop=(j == nk - 1), reads=[ptn, 'vaug'], writes=[pvn])

                    def emit_fin(n_):
                        ao = ao_[n_ % 2]
                        aon = 'aox%d' % (n_ % 2)
                        for hb, (pvx, pvxn) in enumerate(((pvA, pvAn), (pvB, pvBn))):
                            pv = pvx[:, 0:264].rearrange("p (h d) -> p h d", d=66)
                            P.I('dve', 'tensor_tensor', out=den[:, hb * 4:(hb + 1) * 4, :], in0=pv[:, :, 64:65],
                                in1=es_t[:, hb * 4:(hb + 1) * 4, :], op=ALU.add, reads=[pvxn, 'es_t'], writes=['den'])
                        P.I('dve', 'reciprocal', out=rec[:], in_=den[:], reads=['den'], writes=['rec'])
                        for hb, (pvx, pvxn) in enumerate(((pvA, pvAn), (pvB, pvBn))):
                            pv = pvx[:, 0:264].rearrange("p (h d) -> p h d", d=66)
                            P.I('dve', 'tensor_tensor', out=ao[:, hb * 4:(hb + 1) * 4, :], in0=pv[:, :, 0:64],
                                in1=rec[:, hb * 4:(hb + 1) * 4, :].to_broadcast([128, 4, 64]), op=ALU.mult,
                                reads=[pvxn, 'rec'], writes=[aon])

                    def emit_tr(n_):
                        ao = ao_[n_ % 2]
                        aon = 'aox%d' % (n_ % 2)
                        trn = 'BK%d' % (2 + n_ % 2)
                        trb = BKb[2 + n_ % 2][:, 0:512].rearrange("p (c t) -> p c t", t=128)
                        ao2 = ao[:].rearrange("p h d -> p (h d)")
                        for c4 in range(4):
                            P.I('pe', 'transpose', trb[:, c4, :], ao2[:, c4 * 128:(c4 + 1) * 128], ident_b[:],
                                reads=[aon, 'ident_b'], writes=[trn])
                        P.I('act', 'activation', out=mixT[:, 0:4, n_ * 128:(n_ + 1) * 128], in_=trb, func=AF.Copy,
                            reads=[trn], writes=['mixT_a%d' % n_])

                    items = [(n_, hq) for n_ in range(16) for hq in range(8)]
                    pend_tr = []
                    if OPAIR:
                        ptp = [sb(ea, "ptp%d" % i, [128, 640], BF16) for i in range(6)]
                        pitems = [(n_, c_) for n_ in range(16) for c_ in range(4)]

                        def pair_scores(n_, c_, slot):
                            kbs = kbs_of(n_)
                            nk = len(kbs)
                            kvh = c_ // 2
                            for j, (kt, mi) in enumerate(kbs):
                                for par in range(2):
                                    rows = slice(par * 64, par * 64 + 64)
                                    P.I('pe', 'matmul', PP[par][:, j * 128:(j + 1) * 128], kT[rows, kvh, kt * 128:(kt + 1) * 128],
                                        qT[rows, c_, n_ * 128:(n_ + 1) * 128], start=True, stop=True,
                                        reads=['kT', 'qT'], writes=['PP%da' % par, 'PP%db' % par])
                            for par in range(2):
                                pt = ptp[slot * 2 + par]
                                ptn = 'ptp%d' % (slot * 2 + par)
                                P.I('act', 'activation', out=pt[:, 0:nk * 128], in_=PP[par][:, 0:nk * 128], func=AF.Exp,
                                    reads=['PP%da' % par, 'PP%db' % par], writes=[ptn])
                                for j, (kt, mi) in enumerate(kbs):
                                    if mi is not None:
                                        P.I('dve', 'tensor_tensor', out=pt[:, j * 128:(j + 1) * 128], in0=pt[:, j * 128:(j + 1) * 128],
                                            in1=mattn[:, mi, :], op=ALU.mult, reads=[ptn, 'mattn'], writes=[ptn])

                        PL = 2
                        for i in range(len(pitems) + PL):
                            if i < len(pitems):
                                pair_scores(*pitems[i], slot=i % 3)
                            if i >= PL:
                                n_, c_ = pitems[i - PL]
                                for par in range(2):
                                    emit_pv(n_, 2 * c_ + par, pti=None, ptx=(ptp[((i - PL) % 3) * 2 + par], 'ptp%d' % (((i - PL) % 3) * 2 + par)))
                                if c_ == 3:
                                    emit_fin(n_)
                                    pend_tr.append((i + 1, n_))
                            while pend_tr and (pend_tr[0][0] <= i or i == len(pitems) + PL - 1):
                                emit_tr(pend_tr.pop(0)[1])
                        items = []
                    LAG = max(OATT, 2) if OATT else 1
                    NPT = LAG + 1
                    for i in range((len(items) + LAG) if items else 0):
                        if i < len(items):
                            emit_scores(*items[i], pti=(i % NPT if OATT else None))
                        if i >= LAG:
                            n_, hq = items[i - LAG]
                            emit_pv(n_, hq, pti=((i - LAG) % NPT if OATT else None))
                            if hq == 7:
                                emit_fin(n_)
                                pend_tr.append((i + 2, n_))
                        while pend_tr and (pend_tr[0][0] <= i or i == len(items) + LAG - 1):
                            emit_tr(pend_tr.pop(0)[1])
                    P.end_scope()
                if b == 0:
                    dump("mixT", mixT[:], [128, 8, T], BF16, ['mixT_a%d' % i for i in range(16)])
                if stage <= 2.05:
                    continue

                for pair in range(2):
                    with contextlib.ExitStack() as eb:
                        wB = wsl
                        P.DMA('pool', wB[:, :, 0:128], w_in[:, 768 + pair * 128:768 + (pair + 1) * 128].rearrange("(k p) c -> p k c", p=128), writes=['wsl'])
                        P.DMA('pool', wB[:, :, 128:256], w_in[:, 1024 + pair * 128:1024 + (pair + 1) * 128].rearrange("(k p) c -> p k c", p=128), writes=['wsl'])
                        P.DMA('pool', wB[:, :, 256:288], w_in[:, 2304:2336].rearrange("(k p) c -> p k c", p=128), writes=['wsl'])
                        P.DMA('pool', wB[:, :, 288:544], w_in[:, 1280 + pair * 256:1280 + (pair + 1) * 256].rearrange("(k p) c -> p k c", p=128), writes=['wsl'])
                        P.DMA('pool', wB[:, :, 544:800], w_in[:, 1792 + pair * 256:1792 + (pair + 1) * 256].rearrange("(k p) c -> p k c", p=128), writes=['wsl'])
                        qeT = [sb(eb, "qeT%d" % d_, [128, T], BF16) for d_ in range(2)]
                        keT = [sb(eb, "keT%d" % d_, [128, S], BF16) for d_ in range(2)]
                        ketok = [sb(eb, "ketok%d" % d_, [128, 18, 128], BF16) for d_ in range(2)]
                        gv = sb(eb, "gv", [128, 18, 256], BF16)
                        sgg = sb(eb, "sgg", [128, 16, 256], BF16)
                        Sp = [sb(eb, "Sp%d" % d_, [128, 32, 128], BF16) for d_ in range(2)]
                        a_all = [sb(eb, "a_all%d" % d_, [128, 36], F32) for d_ in range(2)]
                        b_all = [sb(eb, "b_all%d" % d_, [128, 36], F32) for d_ in range(2)]
                        er_all = [sb(eb, "er_all%d" % d_, [128, 36], F32) for d_ in range(2)]
                        lrT = sb(eb, "lrT", [32, 512], BF16)
                        e_t2 = [sb(eb, "e_t%d" % i, [128, 512], F32) for i in range(2)]
                        cs_t2 = [sb(eb, "cs_t%d" % i, [128, 512], F32) for i in range(2)]
                        sf_t = sb(eb, "sf_t", [128, 512], F32)
                        dl_t2 = [sb(eb, "dl_t%d" % i, [128, 512], F32) for i in range(2)]
                        eq_t2 = [sb(eb, "eq_t%d" % i, [128, 512], BF16) for i in range(2)]
                        ek_t2 = [sb(eb, "ek_t%d" % i, [128, 512], BF16) for i in range(2)]
                        sgt = sb(eb, "sgt", [128, 256], F32)
                        Sst = [[sb(eb, "Sst%d_%d" % (d_, i), [128, 128], F32) for i in range(3)] for d_ in range(2)]
                        kvs = [sb(eb, "kvs%d" % i, [128, 128], F32) for i in range(4)]
                        asb = sb(eb, "asb", [128, 2, 2, 128], BF16)
                        ssq = sb(eb, "ssq", [128, 4], F32)
                        ojk = sb(eb, "ojk", [128, 128], BF16)
                        og1 = sb(eb, "og1", [128, 2, 128], F32)
                        og2 = sb(eb, "og2", [128, 256], BF16)
                        def ke_transposes(tls):
                            for ti in tls:
                                for d_ in range(2):
                                    ktn = 'BK%d' % (2 + d_)
                                    ktp = BKb[2 + d_][:, 0:128]
                                    P.I('pe', 'transpose', ktp, keT[d_][:, ti * 128:(ti + 1) * 128], ident_b[:],
                                        reads=['keT%d' % d_, 'ident_b'], writes=[ktn])
                                    (P.I('dve', 'tensor_copy', out=ketok[d_][:, ti, :], in_=ktp, reads=[ktn], writes=['ketok%d' % d_]) if OA4 else P.I('act', 'activation', out=ketok[d_][:, ti, :], in_=ktp, func=AF.Copy, reads=[ktn], writes=['ketok%d' % d_]))

                        orders = [list(range(36)), [3, 2, 1, 0] + list(range(35, 3, -1))]
                        kvi = [0]

                        def rec_step(d_, step):
                            n_ = orders[d_][step]
                            Scur, Scn = Sst[d_][step % 3], 'Sst%d_%d' % (d_, step % 3)
                            Snx, Snn = Sst[d_][(step + 1) % 3], 'Sst%d_%d' % (d_, (step + 1) % 3)
                            if n_ >= 4:
                                if OB2:
                                    P.I('pool', 'tensor_scalar', out=Sp[d_][:, n_ - 4, :], in0=Scur[:], scalar1=er_all[d_][:, n_:n_ + 1], scalar2=None,
                                        op0=ALU.mult, reads=[Scn, 'er_all%d' % d_], writes=['Sp%d' % d_])
                                else:
                                    P.I('act', 'activation', out=Sp[d_][:, n_ - 4, :], in_=Scur[:], func=AF.Copy,
                                        scale=er_all[d_][:, n_:n_ + 1], reads=[Scn, 'er_all%d' % d_], writes=['Sp%d' % d_])
                            if step == 35:
                                return
                            ti = n_ // 2
                            cp = n_ % 2
                            kvp, kvpn = banks[6 + (kvi[0] % 2)]
                            kv_, kvn = kvs[kvi[0] % 4], 'kvs%d' % (kvi[0] % 4)
                            kvi[0] += 1
                            for hh in range(2):
                                P.I('pe', 'matmul', kvp[hh * 64:(hh + 1) * 64, 0:128],
                                    ketok[d_][cp * 64:(cp + 1) * 64, ti, hh * 64:(hh + 1) * 64],
                                    gv[cp * 64:(cp + 1) * 64, ti, hh * 128:(hh + 1) * 128], start=True, stop=True,
                                    tile_position=(cp * 64, hh * 64), reads=['ketok%d' % d_, 'gv'], writes=[kvpn])
                            if d_ == 0 or not OPT_REC:
                                if OB3:
                                    P.I('act', 'activation', out=kv_[:], in_=kvp[:, 0:128], func=AF.Copy, scale=b_all[d_][:, n_:n_ + 1],
                                        reads=[kvpn, 'b_all%d' % d_], writes=[kvn])
                                else:
                                    P.I('dve', 'tensor_scalar', out=kv_[:], in0=kvp[:, 0:128], scalar1=b_all[d_][:, n_:n_ + 1], scalar2=None,
                                        op0=ALU.mult, reads=[kvpn, 'b_all%d' % d_], writes=[kvn])
                                P.I('dve', 'scalar_tensor_tensor', out=Snx[:], in0=Scur[:], scalar=a_all[d_][:, n_:n_ + 1], in1=kv_[:],
                                    op0=ALU.mult, op1=ALU.add, reads=[Scn, kvn, 'a_all%d' % d_], writes=[Snn])
                            else:
                                P.I('act', 'activation', out=kv_[:], in_=kvp[:, 0:128], func=AF.Copy, scale=b_all[d_][:, n_:n_ + 1],
                                    reads=[kvpn, 'b_all%d' % d_], writes=[kvn])
                                P.I('pool', 'tensor_scalar', out=Snx[:], in0=Scur[:], scalar1=a_all[d_][:, n_:n_ + 1], scalar2=None,
                                    op0=ALU.mult, reads=[Scn, 'a_all%d' % d_], writes=[Snn])
                                P.I('pool', 'tensor_tensor', out=Snx[:], in0=Snx[:], in1=kv_[:], op=ALU.add,
                                    reads=[Snn, kvn], writes=[Snn])

                        if OFW:
                            for d_ in range(2):
                                P.I('pool', 'memset', Sst[d_][0][:], 0.0, writes=['Sst%d_0' % d_])
                        prev_tiles = None
                        for (s0, n) in chunks:
                            tiles = list(range(s0 // 128, (s0 + n) // 128))
                            hTr = ['hT%d' % i for i in tiles]
                            nch = n // 64
                            c0 = s0 // 64
                            lat = s0 >= C
                            pgq, pgqn = banks[0]
                            pgk, pgkn = banks[1]
                            plr, plrn = banks[2]
                            for kc in range(8):
                                P.I('pe', 'matmul', plr[0:32, 0:n], wB[:, kc, 256:288], hT[:, kc, s0:s0 + n], start=(kc == 0), stop=(kc == 7),
                                    reads=['wsl'] + hTr, writes=[plrn])
                            P.I('act', 'activation', out=lrT[:, 0:n], in_=plr[0:32, 0:n], func=AF.Copy, reads=[plrn], writes=['lrT'])
                            def qk_proj():
                                if lat:
                                    for kc in range(8):
                                        P.I('pe', 'matmul', pgq[:, 0:n], wB[:, kc, 0:128], hT[:, kc, s0:s0 + n], start=(kc == 0), stop=(kc == 7),
                                            reads=['wsl'] + hTr, writes=[pgqn])
                                for kc in range(8):
                                    P.I('pe', 'matmul', pgk[:, 0:n], wB[:, kc, 128:256], hT[:, kc, s0:s0 + n], start=(kc == 0), stop=(kc == 7),
                                        reads=['wsl'] + hTr, writes=[pgkn])
                            cs3s, Ccs, ris, tis, dl3s = {}, {}, {}, {}, {}

                            def s1(d_):
                                pz, pzn = banks[3 - d_]
                                e_ = e_t2[d_]
                                P.I('pe', 'matmul', pz[:, 0:n], wdx[:, d_, pair * 128:(pair + 1) * 128], lrT[:, 0:n], start=True, stop=True,
                                    reads=['wdx', 'lrT'], writes=[pzn])
                                P.I('act', 'activation', out=e_[:, 0:n], in_=pz[:, 0:n], func=AF.Exp, scale=-1.0,
                                    bias=nbd[:, d_, pair:pair + 1], reads=[pzn, 'nbd'], writes=['e_t%d' % d_])
                                P.I('act', 'activation', out=e_[:, 0:n], in_=e_[:, 0:n], func=AF.Ln, bias=1.0,
                                    reads=['e_t%d' % d_], writes=['e_t%d' % d_])

                            def s2(d_):
                                e_ = e_t2[d_]
                                cs_ = cs_t2[d_]
                                P.I('dve', 'tensor_tensor_scan', out=cs_[:, 0:n], data0=rmask[:, 0:n], data1=e_[:, 0:n], initial=0.0,
                                    op0=ALU.mult, op1=ALU.add, reads=['rmask', 'e_t%d' % d_], writes=['cs_t%d' % d_])
                                cs3 = cs_[:, 0:n].rearrange("p (c i) -> p c i", i=64)
                                if d_ == 0:
                                    Cc, Ccn = cs3, 'cs_t0'
                                    ri, ti_ = 31, 63
                                else:
                                    sf3 = sf_t[:, 0:n].rearrange("p (c i) -> p c i", i=64)
                                    P.I('dve', 'tensor_tensor', out=sf_t[:, 0:n], in0=e_[:, 0:n], in1=cs_[:, 0:n], op=ALU.subtract,
                                        reads=['e_t1', 'cs_t1'], writes=['sf_t'])
                                    P.I('dve', 'tensor_tensor', out=sf3, in0=sf3, in1=cs3[:, :, 63:64].to_broadcast([128, nch, 64]),
                                        op=ALU.add, reads=['sf_t', 'cs_t1'], writes=['sf_t'])
                                    Cc, Ccn = sf3, 'sf_t'
                                    ri, ti_ = 32, 0
                                dl3 = dl_t2[d_][:, 0:n].rearrange("p (c i) -> p c i", i=64)
                                P.I('dve', 'tensor_tensor', out=dl3, in0=Cc, in1=Cc[:, :, ri:ri + 1].to_broadcast([128, nch, 64]),
                                    op=ALU.subtract, reads=[Ccn], writes=['dl_t%d' % d_])
                                Ccs[d_], ris[d_], tis[d_], dl3s[d_] = (Cc, Ccn), ri, ti_, dl3

                            def s3(d_):
                                (Cc, Ccn), ri, ti_, dl3 = Ccs[d_], ris[d_], tis[d_], dl3s[d_]
                                dln = 'dl_t%d' % d_
                                P.I('act', 'activation', out=ek_t2[d_][:, 0:n], in_=dl_t2[d_][:, 0:n], func=AF.Exp, scale=1.0 / 16,
                                    reads=[dln], writes=['ek_t%d' % d_])
                                if lat:
                                    P.I('act', 'activation', out=eq_t2[d_][:, 0:n], in_=dl_t2[d_][:, 0:n], func=AF.Exp, scale=-1.0 / 16,
                                        reads=[dln], writes=['eq_t%d' % d_])
                                P.I('act', 'activation', out=a_all[d_][:, c0:c0 + nch].unsqueeze(2), in_=Cc[:, :, ti_:ti_ + 1], func=AF.Exp,
                                    scale=-1.0 / 16, reads=[Ccn], writes=['a_all%d' % d_])
                                P.I('act', 'activation', out=er_all[d_][:, c0:c0 + nch].unsqueeze(2), in_=Cc[:, :, ri:ri + 1], func=AF.Exp,
                                    scale=-1.0 / 16, reads=[Ccn], writes=['er_all%d' % d_])
                                P.I('act', 'activation', out=b_all[d_][:, c0:c0 + nch].unsqueeze(2), in_=dl3[:, :, ti_:ti_ + 1], func=AF.Exp,
                                    scale=-1.0 / 16, reads=[dln], writes=['b_all%d' % d_])

                            def s4(d_):
                                if lat:
                                    P.I('dve', 'scalar_tensor_tensor', out=qeT[d_][:, s0 - C:s0 - C + n], in0=pgq[:, 0:n], scalar=0.125,
                                        in1=eq_t2[d_][:, 0:n], op0=ALU.mult, op1=ALU.mult, reads=[pgqn, 'eq_t%d' % d_], writes=['qeT%d' % d_])
                                P.I('dve', 'tensor_tensor', out=keT[d_][:, s0:s0 + n], in0=pgk[:, 0:n], in1=ek_t2[d_][:, 0:n], op=ALU.mult,
                                    reads=[pgkn, 'ek_t%d' % d_], writes=['keT%d' % d_])

                            if OPT_REORD:
                                for d_ in range(2):
                                    s1(d_)
                                if prev_tiles:
                                    ke_transposes(prev_tiles)
                                    if OFW:
                                        for ti in prev_tiles:
                                            rec_step(0, 2 * ti)
                                            rec_step(0, 2 * ti + 1)
                                qk_proj()
                            else:
                                qk_proj()
                                for d_ in range(2):
                                    s1(d_)
                            for stg in (s2, s3, s4):
                                for d_ in range(2):
                                    stg(d_)
                            prev_tiles = tiles
                            for ti in (tiles if BIS >= 5 else []):
                                pgv, pgvn = banks[4 + (ti % 2)]
                                for kc in range(8):
                                    P.I('pe', 'matmul', pgv[:, 0:256], hT[:, kc, ti * 128:(ti + 1) * 128], wB[:, kc, 288:544],
                                        start=(kc == 0), stop=(kc == 7), reads=['wsl', 'hT%d' % ti], writes=[pgvn])
                                (P.I('dve', 'tensor_copy', out=gv[:, ti, :], in_=pgv[:, 0:256], reads=[pgvn], writes=['gv']) if OB1 else P.I('act', 'activation', out=gv[:, ti, :], in_=pgv[:, 0:256], func=AF.Copy, reads=[pgvn], writes=['gv']))
                                if lat:
                                    for kc in range(8):
                                        P.I('pe', 'matmul', pgv[:, 256:512], hT[:, kc, ti * 128:(ti + 1) * 128], wB[:, kc, 544:800],
                                            start=(kc == 0), stop=(kc == 7), reads=['wsl', 'hT%d' % ti], writes=[pgvn])
                                    P.I('act', 'activation', out=sgt[:], in_=pgv[:, 256:512], func=AF.Silu, reads=[pgvn], writes=['sgt'])
                                    P.I('pool' if OA6 else 'dve', 'tensor_tensor', out=sgg[:, ti - 2, :].rearrange("p (h d) -> p h d", d=128),
                                        in0=sgt[:].rearrange("p (h d) -> p h d", d=128),
                                        in1=glab[:].unsqueeze(1).to_broadcast([128, 2, 128]), op=ALU.mult,
                                        reads=['sgt', 'glab'], writes=['sgg'])
                            if not OPT_REORD:
                                ke_transposes(tiles)
                        if OPT_REORD:
                            ke_transposes(prev_tiles)
                        if pair == 1 and stage > 3 and OPT_CPREP:
                            c_prep(b)
                        if not OFW:
                            for d_ in range(2):
                                P.I('pool', 'memset', Sst[d_][0][:], 0.0, writes=['Sst%d_0' % d_])
                        if not OFW:
                            for step in range(36 if stage >= 2.4 else 0):
                                for d_ in range(2):
                                    rec_step(d_, step)
                        else:
                            for ti in prev_tiles:
                                rec_step(0, 2 * ti)
                                rec_step(0, 2 * ti + 1)
                            for step in range(36):
                                rec_step(1, step)

                        asb2 = [asb, sb(eb, "asbb", [128, 2, 2, 128], BF16)] + ([sb(eb, "asbc", [128, 2, 2, 128], BF16)] if OG2 else [])
                        NAS = len(asb2)
                        og2b = [og2, sb(eb, "og2b", [128, 256], BF16)]

                        def g_partA(tt):
                            st_ = tt + 2
                            apn = ['PP0a', 'PP0b']
                            aps = PP[0][:, :].rearrange("p (h x) -> p h x", h=2)[:, :, 0:256].rearrange("p h (d i) -> p h d i", d=2)
                            for hh in range(2):
                                rows = slice(hh * 64, hh * 64 + 64)
                                for d_ in range(2):
                                    P.I('pe', 'matmul', aps[:, hh, d_, :], keT[d_][rows, st_ * 128:(st_ + 1) * 128],
                                        qeT[d_][rows, tt * 128:(tt + 1) * 128], start=True, stop=True,
                                        reads=['keT%d' % d_, 'qeT%d' % d_], writes=[apn[hh]])
                            P.I('dve', 'tensor_tensor', out=asb2[tt % NAS][:], in0=aps, in1=mgla[:].unsqueeze(1).to_broadcast([128, 2, 2, 128]),
                                op=ALU.mult, reads=apn + ['mgla'], writes=['asb%d' % (tt % NAS)])

                        def g_partB(tt):
                            st_ = tt + 2
                            asb_ = asb2[tt % NAS]
                            asn = 'asb%d' % (tt % NAS)
                            opn = ['PP1a', 'PP1b']
                            ops_ = PP[1][:, :].rearrange("p (h x) -> p h x", h=2)[:, :, 0:128]
                            for hh in range(2):
                                rows = slice(hh * 64, hh * 64 + 64)
                                for d_ in range(2):
                                    P.I('pe', 'matmul', ops_[:, hh, :], asb_[:, hh, d_, :], gv[:, st_, hh * 128:(hh + 1) * 128],
                                        start=(d_ == 0), stop=False, reads=[asn, 'gv'], writes=[opn[hh]])
                                for cp in range(2):
                                    ch_ = tt * 2 + cp
                                    for d_ in range(2):
                                        last = (d_ == 1)
                                        P.I('pe', 'matmul', ops_[cp * 64:(cp + 1) * 64, hh, :],
                                            qeT[d_][rows, ch_ * 64:(ch_ + 1) * 64], Sp[d_][rows, ch_, :],
                                            start=False, stop=last, tile_position=(hh * 64, cp * 64),
                                            reads=['qeT%d' % d_, 'Sp%d' % d_], writes=[opn[hh]])
                            for hh in range(2):
                                P.I('act', 'activation', out=ojk[:], in_=ops_[:, hh, :], func=AF.Square, accum_out=ssq[:, hh:hh + 1],
                                    reads=[opn[hh]], writes=['ojk', 'ssq'])
                            P.I('act', 'activation', out=ssq[:, 2:4], in_=ssq[:, 0:2], func=AF.Ln, scale=1.0 / 128, bias=EPS,
                                reads=['ssq'], writes=['ssq'])
                            P.I('act', 'activation', out=ssq[:, 0:2], in_=ssq[:, 2:4], func=AF.Exp, scale=-0.5, reads=['ssq'], writes=['ssq'])
                            P.I('dve', 'tensor_tensor', out=og1[:], in0=ops_, in1=ssq[:, 0:2].unsqueeze(2).to_broadcast([128, 2, 128]),
                                op=ALU.mult, reads=opn + ['ssq'], writes=['og1'])
                            P.I('pool' if OB4 else 'dve', 'tensor_tensor', out=og2b[tt % 2][:], in0=og1[:].rearrange("p h v -> p (h v)"), in1=sgg[:, tt, :], op=ALU.mult,
                                reads=['og1', 'sgg'], writes=['og2_%d' % (tt % 2)])

                        def g_partC(tt):
                            trn = 'BK%d' % (tt % 2)
                            trb = BKb[tt % 2][:, 0:256].rearrange("p (c t) -> p c t", t=128)
                            for c2 in range(2):
                                P.I('pe', 'transpose', trb[:, c2, :], og2b[tt % 2][:, c2 * 128:(c2 + 1) * 128], ident_b[:],
                                    reads=['og2_%d' % (tt % 2), 'ident_b'], writes=[trn])
                            P.I('act', 'activation', out=mixT[:, 4 + 2 * pair:6 + 2 * pair, tt * 128:(tt + 1) * 128], in_=trb, func=AF.Copy,
                                reads=[trn], writes=['mixT_g%d_%d' % (pair, tt)])

                        NTT = 16 if stage >= 2.7 else 0
                        GL = 2 if OG2 else 1
                        for i in range(NTT + GL + 1):
                            if i < NTT:
                                g_partA(i)
                            if GL <= i < NTT + GL:
                                g_partB(i - GL)
                            if GL + 1 <= i:
                                g_partC(i - GL - 1)
                        P.end_scope()
                if b == 0:
                    dump("mixT2", mixT[:], [128, 8, T], BF16, ['mixT_g1_%d' % i for i in range(16)])
                if stage <= 3:
                    continue

                with contextlib.ExitStack() as ec:
                    wO = wsl
                    sc2bc = sb(ec, "sc2bc", [128, D], F32)
                    sh2bc = sb(ec, "sh2bc", [128, D], F32)
                    n2gb = sb(ec, "n2gb", [128, D], F32)
                    P.DMA('sp', n2gb[:], n2g.partition_broadcast(128), writes=['n2gb'])
                    if not OPT_CPREP:
                        c_prep(b)
                    bcast_row(sc2bc, 'sc2bc', 32, b)
                    bcast_row(sh2bc, 'sh2bc', 24, b)
                    P.I('dve', 'tensor_scalar', out=sc2bc[:], in0=sc2bc[:], scalar1=1.0, scalar2=None, op0=ALU.add,
                        reads=['sc2bc'], writes=['sc2bc'])
                    P.I('dve', 'tensor_tensor', out=sc2bc[:], in0=sc2bc[:], in1=n2gb[:], op=ALU.mult, reads=['sc2bc', 'n2gb'], writes=['sc2bc'])
                    xr = [sb(ec, "xr%d" % i, [128, D], F32) for i in range(3 + OCL)]
                    x1t = [sb(ec, "x1t%d" % i, [128, D], F32) for i in range(3 + OCL)]
                    h2t = [sb(ec, "h2t%d" % i, [128, D], F32) for i in range(3 + OCL)]
                    h2Tb = [sb(ec, "h2T%d" % i, [128, 8, 128], F32) for i in range(2 if OC8 else 1)]
                    sqj2 = sb(ec, "sqj2", [128, D], BF16)
                    s4 = [sb(ec, "s4_%d" % i, [128, 8], F32) for i in range(3 + OCL)]
                    ex = sb(ec, "ex", [128, NE], F32)
                    lgs = sb(ec, "lgs", [NE, 128], F32)

                    def c_partA(tt):
                        i2 = tt % (3 + OCL)
                        xr_, xrn = xr[i2], 'xr%d' % i2
                        x1_, x1n = x1t[i2], 'x1t%d' % i2
                        h2_, h2n = h2t[i2], 'h2t%d' % i2
                        s4_, s4n = s4[i2], 's4_%d' % i2
                        P.DMA('sp', xr_[:], x[b, tt * 128:(tt + 1) * 128, :], writes=[xrn])
                        for half in range(2):
                            po, pon = banks[half]
                            for kc in range(8):
                                P.I('pe', 'matmul', po, mixT[:, kc, tt * 128:(tt + 1) * 128], wO[:, kc, half * 512:(half + 1) * 512],
                                    start=(kc == 0), stop=(kc == 7),
                                    reads=['wsl', 'mixT_a%d' % tt, 'mixT_g0_%d' % tt, 'mixT_g1_%d' % tt], writes=[pon])
                            P.I('dve', 'tensor_tensor', out=x1_[:, half * 512:(half + 1) * 512], in0=po, in1=xr_[:, half * 512:(half + 1) * 512],
                                op=ALU.add, reads=[pon, xrn], writes=[x1n])
                        P.DMA('sp', out[b, tt * 128:(tt + 1) * 128, :], x1_[:], reads=[x1n], writes=['out_x1'])
                        P.I('act', 'activation', out=sqj2[:], in_=x1_[:], func=AF.Square, accum_out=s4_[:, 0:1], reads=[x1n], writes=['sqj2', s4n])
                        P.I('act', 'activation', out=s4_[:, 1:2], in_=s4_[:, 0:1], func=AF.Ln, scale=1.0 / D, bias=EPS, reads=[s4n], writes=[s4n])
                        P.I('act', 'activation', out=s4_[:, 2:3], in_=s4_[:, 1:2], func=AF.Exp, scale=-0.5, reads=[s4n], writes=[s4n])
                        if OC7:
                            return
                        c_partA2(tt)

                    def c_partA2(tt):
                        i2 = tt % (3 + OCL)
                        x1_, x1n = x1t[i2], 'x1t%d' % i2
                        h2_, h2n = h2t[i2], 'h2t%d' % i2
                        s4_, s4n = s4[i2], 's4_%d' % i2
                        P.I('dve', 'scalar_tensor_tensor', out=h2_[:], in0=x1_[:], scalar=s4_[:, 2:3], in1=sc2bc[:], op0=ALU.mult, op1=ALU.mult,
                            reads=[x1n, s4n, 'sc2bc'], writes=[h2n])
                        P.I('dve' if OA5 else 'pool', 'tensor_tensor', out=h2_[:], in0=h2_[:], in1=sh2bc[:], op=ALU.add, reads=[h2n, 'sh2bc'], writes=[h2n])
                        P.DMA('pool', h2d[b * T + tt * 128:b * T + (tt + 1) * 128, :], h2_[:], reads=[h2n], writes=['h2d'])

                    def c_partB(tt):
                        i2 = tt % (3 + OCL)
                        h2_, h2n = h2t[i2], 'h2t%d' % i2
                        s4_, s4n = s4[i2], 's4_%d' % i2
                        for half in range(2):
                            trf, trfn = banks[2 + half]
                            for q4 in range(4):
                                kc = half * 4 + q4
                                P.I('pe', 'transpose', trf[:, q4 * 128:(q4 + 1) * 128], h2_[:, kc * 128:(kc + 1) * 128], ident_f[:],
                                    reads=[h2n, 'ident_f'], writes=[trfn])
                            h2T = h2Tb[tt % 2] if OC8 else h2Tb[0]
                            P.I('dve', 'tensor_copy', out=h2T[:, half * 4:(half + 1) * 4, :].rearrange("p k t -> p (k t)"), in_=trf,
                                reads=[trfn], writes=['h2T%d' % (tt % 2 if OC8 else 0)])
                        if OC8:
                            return
                        c_partB2(tt)

                    def c_partB2(tt):
                        i2 = tt % (3 + OCL)
                        s4_, s4n = s4[i2], 's4_%d' % i2
                        h2T = h2Tb[tt % 2] if OC8 else h2Tb[0]
                        h2Tn = 'h2T%d' % (tt % 2 if OC8 else 0)
                        lg, lgn = banks[4 + tt % 2]
                        for kc in range(8):
                            P.I('pe', 'matmul', lg[0:NE, 128:256], wr[:, kc, :], h2T[:, kc, :], start=(kc == 0), stop=(kc == 7),
                                reads=[h2Tn, 'wr'], writes=[lgn])
                        P.I('act', 'activation', out=lgs[:], in_=lg[0:NE, 128:256], func=AF.Copy, reads=[lgn], writes=['lgs'])
                        P.I('pe', 'transpose', lg[:, 0:NE], lgs[:], ident_f[0:NE, 0:NE], reads=['lgs', 'ident_f'], writes=[lgn])
                        P.I('dve', 'tensor_reduce', out=s4_[:, 3:4], in_=lg[:, 0:NE], axis=AX.X, op=ALU.max, negate=True, reads=[lgn], writes=[s4n])
                        P.I('act', 'activation', out=ex[:], in_=lg[:, 0:NE], func=AF.Exp, bias=s4_[:, 3:4], accum_out=s4_[:, 4:5],
                            reads=[lgn, s4n], writes=['ex', s4n])
                        P.I('dve', 'reciprocal', out=s4_[:, 5:6], in_=s4_[:, 4:5], reads=[s4n], writes=[s4n])
                        P.I('dve', 'tensor_scalar', out=aff[:, b, tt, :], in0=ex[:], scalar1=s4_[:, 5:6], scalar2=None, op0=ALU.mult,
                            reads=['ex', s4n], writes=['aff'])

                    for tt in range(21):
                        if tt < 16:
                            c_partA(tt)
                        if 2 + OCL <= tt < 18 + OCL:
                            c_partB(tt - 2 - OCL)
                        if OC8 and 3 + OCL <= tt < 19 + OCL:
                            c_partB2(tt - 3 - OCL)
                        if OC7 and not OC9 and tt < 16:
                            c_partA2(tt)
                        if OC7 and OC9 and 1 <= tt < 17:
                            c_partA2(tt - 1)
                    P.DMA('sp', affd[b * T:(b + 1) * T, :].rearrange("(t p) e -> p t e", p=128), aff[:, b, :, :], reads=['aff'], writes=['affd'])
                    atmp = sb(ec, "atmp", [NE, T], F32)
                    for g4 in range(4):
                        tb, tbn = banks[6 + (g4 % 2)]
                        for q4 in range(4):
                            tt = g4 * 4 + q4
                            P.I('pe', 'transpose', tb[0:NE, q4 * 128:(q4 + 1) * 128], aff[:, b, tt, :], ident_f[:],
                                reads=['aff', 'ident_f'], writes=[tbn])
                        P.I('act', 'activation', out=atmp[:, g4 * 512:(g4 + 1) * 512], in_=tb[0:NE, :], func=AF.Copy, reads=[tbn], writes=['atmp'])
                    P.DMA('sp', affT[b * NE:(b + 1) * NE, :], atmp[:], reads=['atmp'], writes=['affT'])
                    P.end_scope()
            dump("aff", aff[:], [128, NB, 16, NE], F32, ['aff'])
            dump("affT", affT[:], [R, T], F32, ['affT'])
            P.barrier()

        if stage >= 5:
            idx_i = sb(es, "idx_i", [128, R, 2], I32)
            g2bc = sb(es, "g2bc", [128, NB, D], F32)
            wg = [sb(es, "wg%d" % i, [128, 8, D], BF16) for i in range(2)]
            wu = [sb(es, "wu%d" % i, [128, 8, D], BF16) for i in range(2)]
            wd = [sb(es, "wd%d" % i, [128, 8, D], BF16) for i in range(2)]

            def load_w(e):
                i2 = e % 2
                for (dst, src, nm) in ((wg[i2], weg, 'wg%d' % i2), (wu[i2], weu, 'wu%d' % i2), (wd[i2], wed, 'wd%d' % i2)):
                    for k2 in range(4):
                        P.DMA('poolw', dst[:, 2 * k2:2 * k2 + 2, :], src[e, k2 * 256:(k2 + 1) * 256, :].rearrange("(k p) f -> p k f", p=128),
                              writes=['%s_k%d' % (nm, k2)])


            load_w(0)
            load_w(1)
            with contextlib.ExitStack() as er:
                rep2 = [sb(er, "repb%d" % i, [128, 128], F32) for i in range(2)]
                for b in range(NB):
                    for half in range(2):
                        bk, bkn = banks[6 + half]
                        for q4 in range(4):
                            kc = half * 4 + q4
                            r_ = rep2[q4 % 2]
                            rn = 'repb%d' % (q4 % 2)
                            P.I('dve', 'tensor_copy', out=r_[:], in_=modT[:, 40 + kc, b:b + 1].to_broadcast([128, 128]), reads=['modT'], writes=[rn])
                            P.I('pe', 'matmul', bk[:, q4 * 128:(q4 + 1) * 128], r_[:], ident_f[:], start=True, stop=True,
                                reads=[rn, 'ident_f'], writes=[bkn])
                        P.I('act', 'activation', out=g2bc[:, b, half * 512:(half + 1) * 512], in_=bk, func=AF.Copy, reads=[bkn], writes=['g2bc'])
                lo = sb(er, "lo", [R, 1], F32)
                hi = sb(er, "hi", [R, 1], F32)
                mid = sb(er, "mid", [R, 1], F32)
                cntt = sb(er, "cntt", [R, 1], F32)
                sel = sb(er, "sel", [R, 1], F32)
                tmpb = sb(er, "tmpb", [R, 1], F32)
                junk = sb(er, "junk", [128, T], BF16)
                mask = sb(er, "mask", [R, T], F32)
                cum = sb(er, "cum", [R, T], F32)
                zer = sb(er, "zer", [R, T], F32)
                P.I('pool', 'memset', zer[:], 0.0, writes=['zer'])
                slot = sb(er, "slot", [128, 2], F32)
                idxf = sb(er, "idxf", [128, R, 2], F32)
                NCB = 3 if OPT_I16 else 2
                cb = [sb(er, "cb%d" % i, [128, T], I16 if OPT_I16 else F32) for i in range(NCB)]
                junk2 = sb(er, "junk2", [128, T], BF16)
                sloth = sb(er, "sloth", [128, 2], F32)
                P.DMA('sp', slot[:], c_slotid, writes=['slot'])
                P.I('dve', 'tensor_scalar', out=sloth[:], in0=slot[:], scalar1=0.5, scalar2=None, op0=ALU.add, reads=['slot'], writes=['sloth'])
                P.I('dve', 'memset', lo[:], 0.0, writes=['lo'])
                P.I('dve', 'memset', hi[:], 1.0, writes=['hi'])
                for it in range(30):
                    P.I('dve', 'tensor_tensor', out=mid[:], in0=lo[:], in1=hi[:], op=ALU.add, reads=['lo', 'hi'], writes=['mid'])
                    P.I('dve', 'tensor_scalar', out=mid[:], in0=mid[:], scalar1=0.5, scalar2=None, op0=ALU.mult, reads=['mid'], writes=['mid'])
                    P.I('dve', 'tensor_scalar', out=junk[0:R, :], in0=affT[:], scalar1=mid[:, 0:1], scalar2=0.0, op0=ALU.is_ge, op1=ALU.add,
                        accum_out=cntt[:, 0:1], reads=['affT', 'mid'], writes=['junk', 'cntt'])
                    P.I('dve', 'tensor_scalar', out=sel[:], in0=cntt[:], scalar1=float(CAP) - 0.5, scalar2=None, op0=ALU.is_ge,
                        reads=['cntt'], writes=['sel'])
                    P.I('dve', 'scalar_tensor_tensor', out=lo[:], in0=mid[:], scalar=sel[:, 0:1], in1=lo[:], op0=ALU.mult, op1=ALU.max,
                        reads=['mid', 'sel', 'lo'], writes=['lo'])
                    P.I('dve', 'scalar_tensor_tensor', out=tmpb[:], in0=sel[:], scalar=2.0, in1=mid[:], op0=ALU.mult, op1=ALU.add,
                        reads=['mid', 'sel'], writes=['tmpb'])
                    P.I('dve', 'tensor_tensor', out=hi[:], in0=tmpb[:], in1=hi[:], op=ALU.min, reads=['tmpb', 'hi'], writes=['hi'])
                P.I('dve', 'tensor_scalar', out=mask[:], in0=affT[:], scalar1=lo[:, 0:1], scalar2=None, op0=ALU.is_ge,
                    reads=['affT', 'lo'], writes=['mask'])
                P.I('dve', 'tensor_tensor_scan', out=cum[:], data0=zer[:], data1=mask[:], initial=0.0, op0=ALU.add, op1=ALU.add,
                    reads=['mask', 'zer'], writes=['cum'])
                if OPT_RT:
                    ohr = [sb(er, "ohr%d" % i, [R, 128], F32) for i in range(2)]
                    idxp = sb(er, "idxp", [128, R, 2, 4], F32)
                    for r in range(R):
                        oh, ohn = ohr[r % 2], 'ohr%d' % (r % 2)
                        P.I('dve', 'tensor_copy', out=oh[:], in_=ident_f[0:R, r:r + 1].to_broadcast([R, 128]), reads=['ident_f'], writes=[ohn])
                        for c4 in range(4):
                            bk, bkn = banks[(r % 2) * 4 + c4]
                            P.I('pe', 'matmul', bk, oh[:], cum[:, c4 * 512:(c4 + 1) * 512], start=True, stop=True,
                                reads=[ohn, 'cum'], writes=[bkn])
                            for st_ in range(2):
                                if c4 < 2:
                                    P.I('dve', 'tensor_scalar', out=junk[:, 0:512], in0=bk, scalar1=slot[:, st_:st_ + 1], scalar2=0.0, op0=ALU.is_le,
                                        op1=ALU.add, accum_out=idxp[:, r, st_, c4:c4 + 1], reads=[bkn, 'slot'], writes=['junk', 'idxp'])
                                else:
                                    P.I('act', 'activation', out=junk2[:, 0:512], in_=bk, func=AF.Sign, scale=-1.0, bias=sloth[:, st_:st_ + 1],
                                        accum_out=idxp[:, r, st_, c4:c4 + 1], reads=[bkn, 'sloth'], writes=['junk2', 'idxp_a'])
                    P.I('dve', 'tensor_scalar', out=idxp[:, :, :, 2:4], in0=idxp[:, :, :, 2:4], scalar1=0.5, scalar2=256.0, op0=ALU.mult, op1=ALU.add,
                        reads=['idxp', 'idxp_a'], writes=['idxp', 'idxp_a'])
                    P.I('dve', 'tensor_reduce', out=idxf[:].rearrange("p r s -> p (r s)"), in_=idxp[:].rearrange("p r s c -> p (r s) c"),
                        axis=AX.X, op=ALU.add, reads=['idxp', 'idxp_a'], writes=['idxf', 'idxf_a'])
                else:
                    if OPT_I16:
                        cum16 = sb(er, "cum16", [R, T], I16)
                        P.I('dve', 'tensor_copy', out=cum16[:], in_=cum[:], reads=['cum'], writes=['cum16'])
                        P.DMA('sp', cumd, cum16[:], reads=['cum16'], writes=['cumd'])
                    else:
                        P.DMA('sp', cumd, cum[:], reads=['cum'], writes=['cumd'])
                    for r in range(R):
                        b = r // NE
                        cb_, cbn = cb[r % NCB], 'cb%d' % (r % NCB)
                        P.DMA('sp', cb_[:], cumd[r:r + 1, :].partition_broadcast(128), reads=['cumd'], writes=[cbn])
                        P.I('dve', 'tensor_scalar', out=junk[:], in0=cb_[:], scalar1=slot[:, 0:1], scalar2=0.0, op0=ALU.is_le, op1=ALU.add,
                            accum_out=idxf[:, r, 0:1], reads=[cbn, 'slot'], writes=['junk', 'idxf'])
                        P.I('act', 'activation', out=junk2[:], in_=cb_[:], func=AF.Sign, scale=-1.0, bias=sloth[:, 1:2],
                            accum_out=idxf[:, r, 1:2], reads=[cbn, 'sloth'], writes=['junk2', 'idxf_a'])
                if not OPT_RT:
                    P.I('dve', 'tensor_scalar', out=idxf[:, :, 1:2], in0=idxf[:, :, 1:2], scalar1=0.5, scalar2=float(T) / 2, op0=ALU.mult, op1=ALU.add,
                        reads=['idxf', 'idxf_a'], writes=['idxf'])
                for b in range(NB):
                    if b > 0:
                        P.I('dve', 'tensor_scalar', out=idxf[:, b * NE:(b + 1) * NE, :], in0=idxf[:, b * NE:(b + 1) * NE, :], scalar1=float(b * T),
                            scalar2=None, op0=ALU.add, reads=['idxf'], writes=['idxf'])
                P.I('dve', 'tensor_copy', out=idx_i[:], in_=idxf[:], reads=['idxf'], writes=['idx_i'])
                dump("idxf", idxf[:], [128, R, 2], F32, ['idxf'])
                P.end_scope()

            with contextlib.ExitStack() as ef:
                xs = sb(ef, "xs", [128, NJ, D], BF16)
                xsT = sb(ef, "xsT", [128, 8, NSL], BF16)
                hidT = sb(ef, "hidT", [128, 8, NSL], BF16)
                gsel = sb(ef, "gsel", [128, NJ, NE], F32)
                sgm = [sb(ef, "sgm%d" % i, [128, 512], F32) for i in range(2)]
                yg = [sb(ef, "yg%d" % i, [128, D], F32) for i in range(3)]
                nn = min(512, NSL)
                nh = NSL // nn

                gsel2 = [gsel, sb(ef, "gselb", [128, NJ, NE], F32)]

                def wnames(nm):
                    return ['%s_k%d' % (nm, k2) for k2 in range(4)]

                def gathers(e):
                    gs = gsel2[e % 2]
                    for j in range(NJ):
                        b = j // 2
                        st_ = j % 2
                        r = b * NE + e
                        P.dma('pool', (lambda eng, o=xs[:, j, :], ix=idx_i[:, r, st_:st_ + 1]: eng.indirect_dma_start(
                            out=o, out_offset=None, in_=h2d, in_offset=bass.IndirectOffsetOnAxis(ap=ix, axis=0))),
                            reads=['idx_i', 'h2d'], writes=['xs%d' % j])
                        P.dma('pool', (lambda eng, o=gs[:, j, :], ix=idx_i[:, r, st_:st_ + 1]: eng.indirect_dma_start(
                            out=o, out_offset=None, in_=affd, in_offset=bass.IndirectOffsetOnAxis(ap=ix, axis=0))),
                            reads=['idx_i', 'affd'], writes=['gsel%d_%d' % (e % 2, j)])

                gathers(0)
                ygi = 0
                for e in range(NE):
                    i2 = e % 2
                    gsel = gsel2[e % 2]
                    if e >= 1 and e + 1 < NE:
                        load_w(e + 1)
                    wgn, wun, wdn = wnames('wg%d' % i2), wnames('wu%d' % i2), wnames('wd%d' % i2)
                    for j in range(NJ):
                        for half in range(2):
                            bi = ((2 * j + half) % 4) if OM1 else half
                            trn = 'BK%d' % bi
                            trb = BKb[bi][:, 0:512].rearrange("p (c t) -> p c t", t=128)
                            for q4 in range(4):
                                kc = half * 4 + q4
                                P.I('pe', 'transpose', trb[:, q4, :], xs[:, j, kc * 128:(kc + 1) * 128], ident_b[:],
                                    reads=['xs%d' % j, 'ident_b'], writes=[trn])
                            if OM1 and half == 1:
                                P.I('dve', 'tensor_copy', out=xsT[:, half * 4:(half + 1) * 4, j * 128:(j + 1) * 128], in_=trb,
                                    reads=[trn], writes=['xsT'])
                            else:
                                P.I('act', 'activation', out=xsT[:, half * 4:(half + 1) * 4, j * 128:(j + 1) * 128], in_=trb, func=AF.Copy,
                                    reads=[trn], writes=['xsT'])
                    if e + 1 < NE:
                        gathers(e + 1)
                    for fc in range(8):
                        for hs in range(nh):
                            k_ = (fc * nh + hs) % 2
                            pg, pgn = banks[0 + k_]
                            pu, pun = banks[2 + k_]
                            for kc in range(8):
                                P.I('pe', 'matmul', pg[:, 0:nn], wg[i2][:, kc, fc * 128:(fc + 1) * 128], xsT[:, kc, hs * nn:(hs + 1) * nn],
                                    start=(kc == 0), stop=(kc == 7), reads=wgn + ['xsT'], writes=[pgn])
                            for kc in range(8):
                                P.I('pe', 'matmul', pu[:, 0:nn], wu[i2][:, kc, fc * 128:(fc + 1) * 128], xsT[:, kc, hs * nn:(hs + 1) * nn],
                                    start=(kc == 0), stop=(kc == 7), reads=wun + ['xsT'], writes=[pun])
                            P.I('act', 'activation', out=sgm[k_][:, 0:nn], in_=pg[:, 0:nn], func=AF.Silu, reads=[pgn], writes=['sgm%d' % k_])
                            P.I('dve', 'tensor_tensor', out=hidT[:, fc, hs * nn:(hs + 1) * nn], in0=sgm[k_][:, 0:nn], in1=pu[:, 0:nn], op=ALU.mult,
                                reads=['sgm%d' % k_, pun], writes=['hidT'])
                    for j in range(NJ):
                        b = j // 2
                        st_ = j % 2
                        r = b * NE + e
                        y_, yn = yg[ygi % 3], 'yg%d' % (ygi % 3)
                        ygi += 1
                        for half in range(2):
                            py, pyn = banks[4 + half]
                            for kc in range(8):
                                P.I('pe', 'matmul', py, hidT[:, kc, j * 128:(j + 1) * 128], wd[i2][:, kc, half * 512:(half + 1) * 512],
                                    start=(kc == 0), stop=(kc == 7), reads=wdn + ['hidT'], writes=[pyn])
                            P.I('dve', 'scalar_tensor_tensor', out=y_[:, half * 512:(half + 1) * 512], in0=py, scalar=gsel[:, j, e:e + 1],
                                in1=g2bc[:, b, half * 512:(half + 1) * 512], op0=ALU.mult, op1=ALU.mult,
                                reads=[pyn, 'gsel%d_%d' % (e % 2, j), 'g2bc'], writes=[yn])
                        P.dma('pool', (lambda eng, i_=y_[:, :], ix=idx_i[:, r, st_:st_ + 1]: eng.indirect_dma_start(
                            out=outf, out_offset=bass.IndirectOffsetOnAxis(ap=ix, axis=0), in_=i_, in_offset=None, compute_op=ALU.add)),
                            reads=[yn, 'idx_i'], writes=['out_b%d' % b])
                P.barrier()
        P.barrier()
        P.emit()
    return nc


_CONSTS = None


def _prep_core(inp, b0, NB):
    global _CONSTS
    if _CONSTS is None:
        _CONSTS = host_consts()
    f = lambda a: np.ascontiguousarray(np.asarray(a, dtype=np.float32))
    cstack = np.concatenate([inp["c"][b0:b0 + NB], inp["c_ctx"][None, :]], axis=0)
    cT = f(cstack.reshape(NB + 1, 8, 128).transpose(2, 1, 0))
    bdec = np.stack([inp["b_decay_fwd"][0].reshape(2, 128).T, inp["b_decay_bwd"][0].reshape(2, 128).T], axis=1)
    m = dict(
        x=f(inp["x"][b0:b0 + NB]), ctx=f(inp["ctx"][b0:b0 + NB]), cT=cT,
        w_mod=f(inp["w_mod"][0]), b_modT=f(inp["b_mod"][0].reshape(48, 128).T), n1gT=f(inp["norm1_g"][0].reshape(8, 128).T),
        w_in=f(inp["w_in"][0]), qng=f(inp["q_norm_g"][0].reshape(64, 1)), kng=f(inp["k_norm_g"][0].reshape(64, 1)),
        sink=f(inp["attn_sink"][0]), wdf=f(inp["w_decay_fwd"][0]), wdb=f(inp["w_decay_bwd"][0]), bdec=f(bdec),
        glag=f(inp["gla_norm_g"][0]), w_out=f(inp["w_out"][0]), n2g=f(inp["norm2_g"][0]), w_router=f(inp["w_router"][0]),
        weg=f(inp["w_e_gate"][0]), weu=f(inp["w_e_up"][0]), wed=f(inp["w_e_down"][0]),
    )
    m.update(_CONSTS)
    return m


def kernel(**inputs):
    inp = {k: np.asarray(v) for k, v in inputs.items()}
    B = inp["x"].shape[0]
    NB = B // NCORES
    nc = build(NB)
    in_maps = [_prep_core(inp, i * NB, NB) for i in range(NCORES)]
    res = run_bass_kernel_spmd(nc, in_maps, core_ids=list(range(NCORES)))
    outs = [np.asarray(r["out"]).reshape(NB, T, D) for r in res.results]
    return np.concatenate(outs, axis=0).astype(np.float32)
```
